# Optimizing a Trainium2 kernel written in Bass

```python
import math
import jax, jax.numpy as jnp
from jax import lax
import numpy as np

D_MODEL = 1024
BATCH = 2
SEQ = 8192
DEPTH = 1

N_META = 16
GROUP_SIZE = 16
SSM_WIDTH = D_MODEL
N_GROUPS = SSM_WIDTH // GROUP_SIZE
STATE = 64
DT_MIN = 0.001
DT_MAX = 0.1
HEAD_DIM = 64
N_Q_HEADS = D_MODEL // HEAD_DIM
N_KV_HEADS = 2
GQA = N_Q_HEADS // N_KV_HEADS
Q_WIDTH = N_Q_HEADS * HEAD_DIM
KV_WIDTH = N_KV_HEADS * HEAD_DIM
WINDOW = 128
ATTN_BLOCK = 128
ROT_DIM = HEAD_DIM // 4
ROPE_THETA = 500000.0
IN_WIDTH = SSM_WIDTH + Q_WIDTH + 2 * KV_WIDTH + 2 * D_MODEL
N_EXPERTS = 32
TOP_K = 4
D_FF = D_MODEL
SWIGLU_LIMIT = 7.0
SWIGLU_ALPHA = 1.702
EXPERT_BLOCK = 128
RMS_EPS = 1e-5
NEG_INF = -1e30

kernel_name = "hybrid_s5_swa_moe_block"


def rmsnorm(x, g):
    x32 = x.astype(jnp.float32)
    y = x32 * lax.rsqrt(jnp.mean(x32 * x32, axis=-1, keepdims=True) + RMS_EPS)
    return (y * g.astype(jnp.float32)).astype(x.dtype)


def partial_rotary(x, cos, sin):
    half = ROT_DIM // 2
    x1, x2, xp = x[..., :half], x[..., half:ROT_DIM], x[..., ROT_DIM:]
    c = cos[None, :, None, :].astype(x.dtype)
    s = sin[None, :, None, :].astype(x.dtype)
    return jnp.concatenate([x1 * c - x2 * s, x2 * c + x1 * s, xp], axis=-1)


def _ssm_combine(e1, e2):
    a1r, a1i, b1r, b1i = e1
    a2r, a2i, b2r, b2i = e2
    return (a2r * a1r - a2i * a1i,
            a2r * a1i + a2i * a1r,
            a2r * b1r - a2i * b1i + b2r,
            a2r * b1i + a2i * b1r + b2i)


def s5_ssm(u, lam_re, lam_im, log_dt, b_re, b_im, c_re, c_im, d):
    bsz, L, _ = u.shape
    ug = u.reshape(bsz, L, N_GROUPS, GROUP_SIZE).astype(jnp.float32)
    lr = lam_re.astype(jnp.float32)
    li = lam_im.astype(jnp.float32)
    dt = jnp.exp(log_dt.astype(jnp.float32))[:, None]
    mag = jnp.exp(dt * lr)
    a_re = mag * jnp.cos(dt * li)
    a_im = mag * jnp.sin(dt * li)
    den = lr * lr + li * li
    nr, ni = a_re - 1.0, a_im
    f_re = (nr * lr + ni * li) / den
    f_im = (ni * lr - nr * li) / den
    br, bi = b_re.astype(jnp.float32), b_im.astype(jnp.float32)
    bb_re = f_re[..., None] * br - f_im[..., None] * bi
    bb_im = f_re[..., None] * bi + f_im[..., None] * br
    bu_re = jnp.einsum('blgh,gph->blgp', ug, bb_re)
    bu_im = jnp.einsum('blgh,gph->blgp', ug, bb_im)
    a_re_t = jnp.broadcast_to(a_re[None, None], (1, L, N_GROUPS, STATE))
    a_im_t = jnp.broadcast_to(a_im[None, None], (1, L, N_GROUPS, STATE))
    _, _, x_re, x_im = lax.associative_scan(_ssm_combine, (a_re_t, a_im_t, bu_re, bu_im), axis=1)
    y = (jnp.einsum('blgp,ghp->blgh', x_re, c_re.astype(jnp.float32))
         - jnp.einsum('blgp,ghp->blgh', x_im, c_im.astype(jnp.float32))
         + d.astype(jnp.float32)[None, None] * ug)
    return y.reshape(bsz, L, SSM_WIDTH).astype(u.dtype)


def sliding_window_attention(q, k, v, sinks):
    bsz, L = q.shape[:2]
    pad = ATTN_BLOCK - N_META
    Lp = L + pad
    nb = Lp // ATTN_BLOCK
    padcfg = ((0, 0), (pad, 0), (0, 0), (0, 0))
    qb = jnp.pad(q, padcfg).reshape(bsz, nb, ATTN_BLOCK, N_KV_HEADS, GQA, HEAD_DIM)
    kb = jnp.pad(k, padcfg).reshape(bsz, nb, ATTN_BLOCK, N_KV_HEADS, HEAD_DIM)
    vb = jnp.pad(v, padcfg).reshape(bsz, nb, ATTN_BLOCK, N_KV_HEADS, HEAD_DIM)
    blkpad = ((0, 0), (1, 0), (0, 0), (0, 0), (0, 0))
    k_band = jnp.concatenate([jnp.pad(kb, blkpad)[:, :-1], kb], axis=2)
    v_band = jnp.concatenate([jnp.pad(vb, blkpad)[:, :-1], vb], axis=2)
    k_meta, v_meta = k[:, :N_META], v[:, :N_META]

    q_idx = jnp.arange(nb)[:, None] * ATTN_BLOCK + jnp.arange(ATTN_BLOCK)[None, :]
    k_idx = jnp.arange(nb)[:, None] * ATTN_BLOCK - ATTN_BLOCK + jnp.arange(2 * ATTN_BLOCK)[None, :]
    qi, ki = q_idx[:, :, None], k_idx[:, None, :]
    band_ok = (ki <= qi) & (qi - ki < WINDOW) & (ki >= pad + N_META)
    meta_ok = jnp.arange(N_META)[None, None, :] <= (q_idx - pad)[:, :, None]
    mask = jnp.concatenate([band_ok, meta_ok], axis=-1)

    scale = 1.0 / math.sqrt(HEAD_DIM)
    s = jnp.concatenate([jnp.einsum('bnqhgd,bnkhd->bnhgqk', qb, k_band),
                         jnp.einsum('bnqhgd,bmhd->bnhgqm', qb, k_meta)], axis=-1)
    s = jnp.where(mask[None, :, None, None], s.astype(jnp.float32) * scale, NEG_INF)
    sink = jnp.broadcast_to(sinks.astype(jnp.float32).reshape(N_KV_HEADS, GQA)[None, None, :, :, None, None],
                            s.shape[:-1] + (1,))
    p = jax.nn.softmax(jnp.concatenate([s, sink], axis=-1), axis=-1)[..., :-1].astype(v.dtype)
    nbk = 2 * ATTN_BLOCK
    o = (jnp.einsum('bnhgqk,bnkhd->bnqhgd', p[..., :nbk], v_band)
         + jnp.einsum('bnhgqm,bmhd->bnqhgd', p[..., nbk:], v_meta))
    return o.reshape(bsz, Lp, Q_WIDTH)[:, pad:]


def moe_ffn(h, w_router, b_router, w_gate_up, b_gate_up, w_down, b_down):
    bsz, L, dm = h.shape
    xt = h.reshape(-1, dm)
    n_tok = xt.shape[0]
    logits = (xt @ w_router + b_router).astype(jnp.float32)
    top_val, top_idx = lax.top_k(logits, TOP_K)
    gates = jax.nn.softmax(top_val, axis=-1)
    n_asg = n_tok * TOP_K
    e_flat = top_idx.reshape(-1).astype(jnp.int32)
    tok_flat = jnp.arange(n_asg, dtype=jnp.int32) // TOP_K
    g_flat = gates.reshape(-1)
    order = jnp.argsort(e_flat)
    e_sorted = e_flat[order]
    counts = jnp.bincount(e_flat, length=N_EXPERTS)
    starts = jnp.cumsum(counts) - counts
    padded = (counts + EXPERT_BLOCK - 1) // EXPERT_BLOCK * EXPERT_BLOCK
    pends = jnp.cumsum(padded)
    pstarts = pends - padded
    dest = pstarts[e_sorted] + (jnp.arange(n_asg, dtype=jnp.int32) - starts[e_sorted])
    n_blocks = -(-n_asg // EXPERT_BLOCK) + N_EXPERTS
    n_slots = n_blocks * EXPERT_BLOCK
    slot_tok = jnp.full((n_slots,), n_tok, jnp.int32).at[dest].set(tok_flat[order])
    slot_gate = jnp.zeros((n_slots,), jnp.float32).at[dest].set(g_flat[order])
    block_expert = jnp.clip(jnp.searchsorted(pends, jnp.arange(n_blocks) * EXPERT_BLOCK, side='right'),
                            0, N_EXPERTS - 1).astype(jnp.int32)
    x_ext = jnp.concatenate([xt, jnp.zeros((1, dm), xt.dtype)], axis=0)

    def run_block(args):
        toks, e = args
        xb = x_ext[toks]
        gu = xb @ w_gate_up[e] + b_gate_up[e]
        g, up = gu[:, :D_FF], gu[:, D_FF:]
        g = jnp.minimum(g, SWIGLU_LIMIT)
        up = jnp.clip(up, -SWIGLU_LIMIT, SWIGLU_LIMIT)
        hid = g * jax.nn.sigmoid(SWIGLU_ALPHA * g) * (up + 1.0)
        return hid @ w_down[e] + b_down[e]

    y_slots = lax.map(run_block, (slot_tok.reshape(n_blocks, EXPERT_BLOCK), block_expert))
    y_slots = y_slots.reshape(n_slots, dm)
    y = jnp.zeros((n_tok + 1, dm), h.dtype).at[slot_tok].add(
        y_slots * slot_gate[:, None].astype(y_slots.dtype))
    return y[:n_tok].reshape(bsz, L, dm)


def setup_inputs(seed: int = 0) -> dict:
    key = jax.random.key(seed)
    ks = jax.random.split(key, 26)
    f32 = jnp.float32
    nrm = lambda k, shape, s: jax.random.normal(k, shape, f32) * s
    lam_im = jnp.broadcast_to(jnp.pi * jnp.arange(STATE, dtype=f32), (DEPTH, N_GROUPS, STATE))
    return {
        "x": nrm(ks[0], (BATCH, SEQ, D_MODEL), 1.0),
        "meta_tokens": nrm(ks[1], (N_META, D_MODEL), 1.0),
        "norm_mix": 1.0 + nrm(ks[2], (DEPTH, D_MODEL), 0.02),
        "w_in": nrm(ks[3], (DEPTH, D_MODEL, IN_WIDTH), D_MODEL ** -0.5),
        "ssm_lam_re": -0.5 * jnp.exp(nrm(ks[4], (DEPTH, N_GROUPS, STATE), 0.01)),
        "ssm_lam_im": lam_im + nrm(ks[5], (DEPTH, N_GROUPS, STATE), 0.01),
        "ssm_log_dt": jax.random.uniform(ks[6], (DEPTH, N_GROUPS), f32, math.log(DT_MIN), math.log(DT_MAX)),
        "ssm_b_re": nrm(ks[7], (DEPTH, N_GROUPS, STATE, GROUP_SIZE), (2 * GROUP_SIZE) ** -0.5),
        "ssm_b_im": nrm(ks[8], (DEPTH, N_GROUPS, STATE, GROUP_SIZE), (2 * GROUP_SIZE) ** -0.5),
        "ssm_c_re": nrm(ks[9], (DEPTH, N_GROUPS, GROUP_SIZE, STATE), STATE ** -0.5),
        "ssm_c_im": nrm(ks[10], (DEPTH, N_GROUPS, GROUP_SIZE, STATE), STATE ** -0.5),
        "ssm_d": nrm(ks[11], (DEPTH, N_GROUPS, GROUP_SIZE), 1.0),
        "w_glu": nrm(ks[12], (DEPTH, SSM_WIDTH, SSM_WIDTH), SSM_WIDTH ** -0.5),
        "b_glu": nrm(ks[13], (DEPTH, SSM_WIDTH), 0.02),
        "attn_sinks": nrm(ks[14], (DEPTH, N_Q_HEADS), 0.5),
        "w_br_ssm": nrm(ks[15], (DEPTH, SSM_WIDTH, D_MODEL), SSM_WIDTH ** -0.5),
        "w_br_attn": nrm(ks[16], (DEPTH, Q_WIDTH, D_MODEL), Q_WIDTH ** -0.5),
        "w_out": nrm(ks[17], (DEPTH, D_MODEL, D_MODEL), D_MODEL ** -0.5),
        "norm_ffn": 1.0 + nrm(ks[18], (DEPTH, D_MODEL), 0.02),
        "w_router": nrm(ks[19], (DEPTH, D_MODEL, N_EXPERTS), D_MODEL ** -0.5),
        "b_router": nrm(ks[20], (DEPTH, N_EXPERTS), 0.01),
        "w_gate_up": nrm(ks[21], (DEPTH, N_EXPERTS, D_MODEL, 2 * D_FF), D_MODEL ** -0.5),
        "b_gate_up": nrm(ks[22], (DEPTH, N_EXPERTS, 2 * D_FF), 0.02),
        "w_down": nrm(ks[23], (DEPTH, N_EXPERTS, D_FF, D_MODEL), D_FF ** -0.5),
        "b_down": nrm(ks[24], (DEPTH, N_EXPERTS, D_MODEL), 0.02),
        "norm_final": 1.0 + nrm(ks[25], (D_MODEL,), 0.02),
    }


def reference(x, meta_tokens, norm_mix, w_in, ssm_lam_re, ssm_lam_im, ssm_log_dt, ssm_b_re, ssm_b_im,
              ssm_c_re, ssm_c_im, ssm_d, w_glu, b_glu, attn_sinks, w_br_ssm, w_br_attn, w_out,
              norm_ffn, w_router, b_router, w_gate_up, b_gate_up, w_down, b_down, norm_final):
    bsz = x.shape[0]
    meta = jnp.broadcast_to(meta_tokens[None].astype(x.dtype), (bsz, N_META, D_MODEL))
    h = jnp.concatenate([meta, x], axis=1)
    L = h.shape[1]
    pos = jnp.arange(L, dtype=jnp.float32)
    inv_freq = ROPE_THETA ** (-jnp.arange(0, ROT_DIM, 2, dtype=jnp.float32) / ROT_DIM)
    ang = pos[:, None] * inv_freq[None, :]
    cos, sin = jnp.cos(ang), jnp.sin(ang)
    splits = np.cumsum([SSM_WIDTH, Q_WIDTH, KV_WIDTH, KV_WIDTH, D_MODEL]).tolist()

    for layer in range(DEPTH):
        hn = rmsnorm(h, norm_mix[layer])
        proj = hn @ w_in[layer]
        u, q, k, v, g_ssm, g_attn = jnp.split(proj, splits, axis=-1)
        y = s5_ssm(u, ssm_lam_re[layer], ssm_lam_im[layer], ssm_log_dt[layer], ssm_b_re[layer],
                   ssm_b_im[layer], ssm_c_re[layer], ssm_c_im[layer], ssm_d[layer])
        z = jax.nn.gelu(y)
        ssm_out = z * jax.nn.sigmoid(z @ w_glu[layer] + b_glu[layer])
        q = partial_rotary(q.reshape(bsz, L, N_Q_HEADS, HEAD_DIM), cos, sin)
        k = partial_rotary(k.reshape(bsz, L, N_KV_HEADS, HEAD_DIM), cos, sin)
        v = v.reshape(bsz, L, N_KV_HEADS, HEAD_DIM)
        attn_out = sliding_window_attention(q, k, v, attn_sinks[layer])
        mix = (jax.nn.sigmoid(g_ssm) * (ssm_out @ w_br_ssm[layer])
               + jax.nn.sigmoid(g_attn) * (attn_out @ w_br_attn[layer]))
        h = h + mix @ w_out[layer]
        h = h + moe_ffn(rmsnorm(h, norm_ffn[layer]), w_router[layer], b_router[layer], w_gate_up[layer],
                        b_gate_up[layer], w_down[layer], b_down[layer])

    return rmsnorm(h, norm_final)[:, N_META:]
```

```python
import contextlib
from contextlib import ExitStack
import numpy as np
import concourse.bass as bass
import concourse.mybir as mybir

F32 = mybir.dt.float32
BF16 = mybir.dt.bfloat16
I32 = mybir.dt.int32
U32 = mybir.dt.uint32
AF = mybir.ActivationFunctionType
ALU = mybir.AluOpType
AX = mybir.AxisListType

SELF_SYNC = True


class Prog:
    ENG = ("pe", "act", "dve", "pool", "sp")

    def __init__(self, nc):
        self.nc = nc
        self.eobj = {"pe": nc.tensor, "act": nc.scalar, "dve": nc.vector,
                     "pool": nc.gpsimd, "sp": nc.sync}
        self.stack = contextlib.ExitStack()
        self.sems = {}
        self.cnt = {}
        self.ops = {e: [] for e in self.ENG}
        self.seen = {e: {} for e in self.ENG}
        self.lastw = {}
        self.readers = {}
        self.nops = 0

    def sem(self, name):
        if name not in self.sems:
            self.sems[name] = self.stack.enter_context(self.nc.semaphore(name))
            self.cnt[name] = 0
        return self.sems[name]

    def sb(self, st, name, shape, dt):
        return st.enter_context(self.nc.sbuf_tensor("s_" + name, list(shape), dt))

    def ps(self, st, name, shape, dt=F32):
        return st.enter_context(self.nc.psum_tensor("p_" + name, list(shape), dt))

    def _deps(self, eng, reads, writes):
        deps = {}
        def add(tok):
            if tok is None:
                return
            s, v = tok
            if s.startswith("d_"):
                v = self.cnt[s]
            if s == "c_" + eng and (eng == "pe" or not SELF_SYNC):
                return
            if deps.get(s, 0) < v:
                deps[s] = v
        for k in reads:
            add(self.lastw.get(k))
        for k in writes:
            add(self.lastw.get(k))
            for t in self.readers.get(k, ()):
                add(t)
        out = []
        seen = self.seen[eng]
        for s, v in deps.items():
            if seen.get(s, 0) < v:
                seen[s] = v
                out.append((s, v))
        return out

    def _commit(self, tok, reads, writes):
        for k in writes:
            self.lastw[k] = tok
            self.readers[k] = []
        for k in reads:
            self.readers.setdefault(k, []).append(tok)
            if len(self.readers[k]) > 64:
                m = {}
                for s, v in self.readers[k]:
                    if m.get(s, 0) < v:
                        m[s] = v
                self.readers[k] = list(m.items())

    def op(self, eng, fn, reads=(), writes=()):
        s = "c_" + eng
        self.sem(s)
        waits = self._deps(eng, reads, writes)
        self.cnt[s] += 1
        tok = (s, self.cnt[s])
        self.ops[eng].append((waits, fn, s, 1))
        self._commit(tok, reads, writes)
        self.nops += 1
        return tok

    def dma(self, q, out, in_, reads=(), writes=(), stream=None, fn=None):
        s = "d_" + (stream or (str(writes[0]) if writes else "misc"))
        s = s.replace(" ", "").replace("'", "").replace(",", "_").replace("(", "").replace(")", "")
        self.sem(s)
        waits = self._deps(q, reads, writes)
        self.cnt[s] += 16
        tok = (s, self.cnt[s])
        if fn is None:
            fn = lambda e, out=out, in_=in_: e.dma_start(out=out, in_=in_)
        self.ops[q].append((waits, fn, s, 16))
        self._commit(tok, reads, writes)
        self.nops += 1
        return tok

    def fence(self):
        allt = [(s, v) for s, v in self.cnt.items() if v > 0]
        for e in self.ENG:
            waits = []
            for s, v in allt:
                if self.seen[e].get(s, 0) < v:
                    self.seen[e][s] = v
                    waits.append((s, v))
            if waits:
                self.ops[e].append((waits, None, None, 0))
        self.lastw.clear()
        self.readers.clear()

    def flush(self):
        ops = self.ops
        sems = self.sems
        eobj = self.eobj

        def replay(name):
            def f(e):
                for waits, fn, s, inc in ops[name]:
                    for (ws, wv) in waits:
                        e.wait_ge(sems[ws], wv)
                    if fn is not None:
                        ins = fn(e)
                        ins.then_inc(sems[s], inc)
            return f
        with self.nc.Block() as block:
            block.tensor(replay("pe"))
            block.scalar(replay("act"))
            block.vector(replay("dve"))
            block.gpsimd(replay("pool"))
            block.sync(replay("sp"))
        self.ops = {e: [] for e in self.ENG}

    def finish(self):
        self.fence()
        self.flush()
        self.stack.close()

from concourse.bass_utils import run_bass_kernel_spmd
import math

NB_CTX = 49
CTX_FAST = True
SUB = {"u", "ssm", "ssmown", "dma", "qkv", "attn"}
NB_OWN = 16
NB = NB_CTX + NB_OWN
CAP = 384
NE = 32
NJ = 11
EPS = 1e-5


def _host_prep(inp):
    f = np.float32
    x = np.asarray(inp["x"], f)
    meta = np.asarray(inp["meta_tokens"], f)
    w_in = np.asarray(inp["w_in"], f)[0]
    chmap = np.full(NJ * 128, -1, np.int64)
    for pc in range(NJ * 128):
        j, r = divmod(pc, 128)
        pi = 3 * j + r // 32
        if r < 96 and pi < 32:
            chmap[pc] = 32 * pi + r % 32
    val = chmap >= 0

    def padcols(w):
        o = np.zeros(w.shape[:-1] + (NJ * 128,), f)
        o[..., val] = w[..., chmap[val]]
        return o

    def padrows(w):
        o = np.zeros((NJ * 128,) + w.shape[1:], f)
        o[val] = w[chmap[val]]
        return o

    def kmaj(w):
        K, N = w.shape
        return np.ascontiguousarray(w.reshape(K // 128, 128, N).transpose(1, 0, 2))

    com = {}
    com["w_u"] = kmaj(padcols(w_in[:, 0:1024]))
    com["w_qkv"] = kmaj(w_in[:, 1024:2304])
    com["w_g"] = kmaj(w_in[:, 2304:4352])
    com["w_glu"] = kmaj(padrows(padcols(np.asarray(inp["w_glu"], f)[0])))
    com["w_brs"] = kmaj(padrows(np.asarray(inp["w_br_ssm"], f)[0]))
    com["w_bra"] = kmaj(np.asarray(inp["w_br_attn"], f)[0])
    com["w_out"] = kmaj(np.asarray(inp["w_out"], f)[0])
    com["w_rt"] = kmaj(np.asarray(inp["w_router"], f)[0])
    com["b_glu"] = np.ascontiguousarray(padcols(np.asarray(inp["b_glu"], f)[0][None])[0].reshape(NJ, 128).T)
    com["d_pad"] = np.ascontiguousarray(padcols(np.asarray(inp["ssm_d"], f)[0].reshape(1, 1024))[0].reshape(NJ, 128).T)
    com["g_mix"] = np.ascontiguousarray(np.asarray(inp["norm_mix"], f)[0].reshape(8, 128).T)
    com["g_ffn"] = np.ascontiguousarray(np.broadcast_to(np.asarray(inp["norm_ffn"], f)[0][None], (128, 1024)))
    com["g_fin"] = np.ascontiguousarray(np.broadcast_to(np.asarray(inp["norm_final"], f)[None], (128, 1024)))
    com["b_rt"] = np.ascontiguousarray(np.broadcast_to(np.asarray(inp["b_router"], f)[0][None], (128, 32)))
    com["sinks"] = np.ascontiguousarray(np.broadcast_to(np.asarray(inp["attn_sinks"], f)[0][None], (128, 16)))
    com["w_gu"] = np.asarray(inp["w_gate_up"], f)[0]
    com["w_dn"] = np.asarray(inp["w_down"], f)[0]
    com["b_gu"] = np.ascontiguousarray(np.asarray(inp["b_gate_up"], f)[0].reshape(32, 16, 128).transpose(2, 0, 1))
    com["b_dn"] = np.ascontiguousarray(np.asarray(inp["b_down"], f)[0])
    lre = np.asarray(inp["ssm_lam_re"], f)[0]; lim = np.asarray(inp["ssm_lam_im"], f)[0]
    ldt = np.asarray(inp["ssm_log_dt"], f)[0]
    bre = np.asarray(inp["ssm_b_re"], f)[0]; bim = np.asarray(inp["ssm_b_im"], f)[0]
    cre = np.asarray(inp["ssm_c_re"], f)[0]; cim = np.asarray(inp["ssm_c_im"], f)[0]
    sig = np.arange(128); sb_ = sig // 64; sp_ = sig % 64
    pi_ = np.arange(32)
    gS = 2 * pi_[None, :] + sb_[:, None]
    com["lre_S"] = np.ascontiguousarray(lre[gS, sp_[:, None]])
    com["lim_S"] = np.ascontiguousarray(lim[gS, sp_[:, None]])
    com["ldt_S"] = np.ascontiguousarray(ldt[gS])
    c32 = np.arange(32); cb = c32 // 16; chh = c32 % 16
    mC = (cb[None, None, :] == sb_[:, None, None])
    com["cre_S"] = np.ascontiguousarray(np.where(mC, cre[gS[:, :, None], chh[None, None, :], sp_[:, None, None]], 0).astype(f))
    com["cim_S"] = np.ascontiguousarray(np.where(mC, cim[gS[:, :, None], chh[None, None, :], sp_[:, None, None]], 0).astype(f))
    com["bre_S"] = np.ascontiguousarray(np.where(mC, bre[gS[:, :, None], sp_[:, None, None], chh[None, None, :]], 0).astype(f))
    com["bim_S"] = np.ascontiguousarray(np.where(mC, bim[gS[:, :, None], sp_[:, None, None], chh[None, None, :]], 0).astype(f))
    com["w_u0"] = kmaj(w_in[:, 0:1024])
    chp = np.arange(128); jj = np.arange(NJ)
    piR = 3 * jj[None, :] + (chp // 32)[:, None]
    vR = (chp[:, None] < 96) & (piR < 32)
    piRc = np.where(vR, piR, 0)
    gR = 2 * piRc[:, :, None] + sb_[None, None, :]
    vR3 = np.broadcast_to(vR[:, :, None], gR.shape)
    com["lre_R"] = np.ascontiguousarray(np.where(vR3, lre[gR, sp_[None, None, :]], -0.5).astype(f))
    com["lim_R"] = np.ascontiguousarray(np.where(vR3, lim[gR, sp_[None, None, :]], 1.0).astype(f))
    com["ldt_R"] = np.ascontiguousarray(np.where(vR3, ldt[gR], math.log(0.01)).astype(f))
    bch = ((chp % 32) // 16)[:, None, None]; hh = (chp % 16)[:, None, None]
    mB = vR3 & (bch == sb_[None, None, :])
    com["bre_R"] = np.ascontiguousarray(np.where(mB, bre[gR, sp_[None, None, :], hh], 0).astype(f))
    com["bim_R"] = np.ascontiguousarray(np.where(mB, bim[gR, sp_[None, None, :], hh], 0).astype(f))
    kq = np.arange(128)
    com["m_cur"] = (kq[:, None] <= kq[None, :]).astype(f)
    com["m_prev"] = (kq[:, None] > kq[None, :]).astype(f)
    com["tri"] = (kq[:, None] < kq[None, :]).astype(f)
    inv_freq = (500000.0 ** (-np.arange(0, 16, 2, dtype=f) / f(16))).astype(f)
    metablk = np.zeros((128, 1024), f); metablk[:16] = meta
    per = []
    for c in range(8):
        b, k = divmod(c, 4)
        seq = np.concatenate([meta, x[b]], 0)
        plen = 16 + 2048 * k
        npad = NB_CTX * 128 - plen
        xl = np.zeros((NB * 128, 1024), f)
        xl[npad:] = seq[:plen + NB_OWN * 128]
        pos = (np.arange(NB * 128) - npad).clip(0).astype(f)
        pos = np.concatenate([np.arange(128, dtype=f), pos])
        ang = pos[:, None] * inv_freq[None, :]
        d = dict(com)
        d["xloc"] = xl
        d["xmeta"] = metablk
        d["rcos"] = np.cos(ang).astype(f).reshape(NB + 1, 128, 8)
        d["rsin"] = np.sin(ang).astype(f).reshape(NB + 1, 128, 8)
        d["m_first"] = com["m_prev"] * f(1.0 if k > 0 else 0.0)
        per.append(d)
    return per


def build_nc(shapes, debug=False, stop=None, nblk=None):
    nc = bass.Bass("TRN2", target_bir_lowering=False)
    D = {}
    for name, shp in shapes.items():
        D[name] = nc.dram_tensor(name, list(shp), F32, kind="ExternalInput").ap()
    out = nc.dram_tensor("out", [NB_OWN * 128, 1024], F32, kind="ExternalOutput").ap()
    NT = NB_OWN * 128

    def scr(name, shape, dt):
        return nc.dram_tensor(name, list(shape), dt, kind="Internal").ap()
    zT_d = scr("zT_d", [NB_OWN, 128, NJ * 128], BF16)
    aT_d = scr("aT_d", [NB_OWN, 128, 1024], BF16)
    hT_d = scr("hT_d", [NB_OWN, 128, 1024], BF16)
    h2_d = scr("h2_d", [NT, 1024], F32)
    Xs_d = scr("Xs_d", [NE * CAP + 128, 1024], BF16)
    Ys_d = scr("Ys_d", [NE * CAP, 1024], F32)
    dbg = {}
    if debug:
        dbg["h2"] = nc.dram_tensor("dbg_h2", [NT, 1024], F32, kind="ExternalOutput").ap()

    p = Prog(nc)
    G = ExitStack()
    ident = p.sb(G, "ident", [128, 128], BF16)
    identf = p.sb(G, "identf", [128, 128], F32)
    p.op("pool", lambda e: e.memset(identf[:], 1.0), writes=["identf"])
    p.op("pool", lambda e: e.affine_select(out=identf[:], in_=identf[:], pattern=[[1, 128]], compare_op=ALU.is_equal,
                                           fill=0.0, base=0, channel_multiplier=-1), reads=["identf"], writes=["identf"])
    p.op("dve", lambda e: e.tensor_copy(out=ident[:], in_=identf[:]), reads=["identf"], writes=["ident"])
    slot_all = p.sb(G, "slot_all", [128, NB_OWN, 4], I32)
    gate_all = p.sb(G, "gate_all", [128, NB_OWN, 4], F32)
    idx_all = p.sb(G, "idx_all", [128, NB_OWN, 4], F32)

    def load(st, name, shape, dt, src, q="sp"):
        t = p.sb(st, name, shape, dt)
        p.dma(q, t[:], src, writes=[name])
        return t

    P1 = ExitStack()
    w_u = load(P1, "w_u", [128, 8, NJ * 128], BF16, D["w_u"], "pool")
    g_mix = load(P1, "g_mix", [128, 8], F32, D["g_mix"])
    d_pad = load(P1, "d_pad", [128, NJ], F32, D["d_pad"])
    m_cur = load(P1, "m_cur", [128, 128], BF16, D["m_cur"], "pool")
    m_prev = load(P1, "m_prev", [128, 128], BF16, D["m_prev"], "pool")
    m_first = load(P1, "m_first", [128, 128], BF16, D["m_first"], "pool")
    esink = load(P1, "esink", [128, 16], F32, D["sinks"])
    p.op("act", lambda e: e.activation(out=esink[:], in_=esink[:], func=AF.Exp), reads=["esink"], writes=["esink"])

    zrow = p.sb(P1, "zrow", [128, 1024], BF16)
    p.op("pool", lambda e: e.memset(zrow[:], 0.0), writes=["zrow"])
    w_qkv = load(P1, "w_qkv", [128, 8, 1280], BF16, D["w_qkv"], "pool")
    BbP = [p.sb(P1, "BbPr", [128, NJ, 3, 128], BF16), p.sb(P1, "BbPi", [128, NJ, 3, 128], BF16)]
    CTP = [p.sb(P1, "CTPr", [128, 32, 128], BF16), p.sb(P1, "CTPi", [128, 32, 128], BF16)]
    for x_ in range(2):
        p.op("pool", lambda e, x_=x_: e.memset(BbP[x_][:], 0.0), writes=["BbP"])
        p.op("pool", lambda e, x_=x_: e.memset(CTP[x_][:], 0.0), writes=["CTP"])
    Xc = [p.sb(P1, "Xcr", [128, 32], F32), p.sb(P1, "Xci", [128, 32], F32)]
    aS = [p.sb(P1, "aSr", [128, 32], F32), p.sb(P1, "aSi", [128, 32], F32)]
    fS = [p.sb(P1, "fSr", [128, 32], F32), p.sb(P1, "fSi", [128, 32], F32)]
    p.op("dve", lambda e: e.memset(Xc[0][:], 0.0), writes=["Xc"])
    p.op("dve", lambda e: e.memset(Xc[1][:], 0.0), writes=["Xc"])

    def dv(fn, r, w, eng="dve"):
        return p.op(eng, fn, reads=r, writes=w)

    def tt(o, a, b, op, r, w, eng="dve"):
        return p.op(eng, lambda e: e.tensor_tensor(out=o, in0=a, in1=b, op=op), reads=r, writes=w)

    def cexp_tables(T, lre_ap, lim_ap, ldt_ap, shape, tag):
        t = {}
        for nm in ("lr", "li", "dt", "th", "s", "c", "t1", "t2", "t3", "mag", "ar", "ai", "fr", "fi", "den"):
            t[nm] = p.sb(T, tag + nm, shape, F32)
        k = [tag]
        p.dma("sp", t["lr"][:], lre_ap, writes=k)
        p.dma("sp", t["li"][:], lim_ap, writes=k)
        p.dma("sp", t["dt"][:], ldt_ap, writes=k)
        p.op("act", lambda e: e.activation(out=t["dt"][:], in_=t["dt"][:], func=AF.Exp), reads=k, writes=k)
        tt(t["th"][:], t["dt"][:], t["li"][:], ALU.mult, k, k)
        tt(t["t1"][:], t["dt"][:], t["lr"][:], ALU.mult, k, k)
        p.op("act", lambda e: e.activation(out=t["mag"][:], in_=t["t1"][:], func=AF.Exp), reads=k, writes=k)
        p.op("act", lambda e: e.activation(out=t["s"][:], in_=t["th"][:], func=AF.Sin, scale=1.0 / 16), reads=k, writes=k)
        p.op("act", lambda e: e.activation(out=t["t1"][:], in_=t["th"][:], func=AF.Sin, scale=1.0 / 32), reads=k, writes=k)
        tt(t["t1"][:], t["t1"][:], t["t1"][:], ALU.mult, k, k)
        dv(lambda e: e.tensor_scalar(out=t["c"][:], in0=t["t1"][:], scalar1=-2.0, scalar2=1.0, op0=ALU.mult, op1=ALU.add), k, k)
        for _ in range(4):
            tt(t["t1"][:], t["c"][:], t["c"][:], ALU.mult, k, k)
            tt(t["t2"][:], t["s"][:], t["s"][:], ALU.mult, k, k)
            tt(t["t3"][:], t["c"][:], t["s"][:], ALU.mult, k, k)
            tt(t["c"][:], t["t1"][:], t["t2"][:], ALU.subtract, k, k)
            dv(lambda e: e.tensor_scalar(out=t["s"][:], in0=t["t3"][:], scalar1=2.0, scalar2=None, op0=ALU.mult), k, k)
        tt(t["ar"][:], t["mag"][:], t["c"][:], ALU.mult, k, k)
        tt(t["ai"][:], t["mag"][:], t["s"][:], ALU.mult, k, k)
        tt(t["t1"][:], t["lr"][:], t["lr"][:], ALU.mult, k, k)
        tt(t["t2"][:], t["li"][:], t["li"][:], ALU.mult, k, k)
        tt(t["den"][:], t["t1"][:], t["t2"][:], ALU.add, k, k)
        dv(lambda e: e.reciprocal(out=t["den"][:], in_=t["den"][:]), k, k)
        dv(lambda e: e.tensor_scalar(out=t["t3"][:], in0=t["ar"][:], scalar1=-1.0, scalar2=None, op0=ALU.add), k, k)
        tt(t["t1"][:], t["t3"][:], t["lr"][:], ALU.mult, k, k)
        tt(t["t2"][:], t["ai"][:], t["li"][:], ALU.mult, k, k)
        tt(t["t1"][:], t["t1"][:], t["t2"][:], ALU.add, k, k)
        tt(t["fr"][:], t["t1"][:], t["den"][:], ALU.mult, k, k)
        tt(t["t1"][:], t["ai"][:], t["lr"][:], ALU.mult, k, k)
        tt(t["t2"][:], t["t3"][:], t["li"][:], ALU.mult, k, k)
        tt(t["t1"][:], t["t1"][:], t["t2"][:], ALU.subtract, k, k)
        tt(t["fi"][:], t["t1"][:], t["den"][:], ALU.mult, k, k)
        return t

    with ExitStack() as T:
        BbT = [p.sb(T, "BbTr", [128, NJ, 128], BF16), p.sb(T, "BbTi", [128, NJ, 128], BF16)]
        tr_ = cexp_tables(T, D["lre_R"].rearrange("p j s -> p (j s)"), D["lim_R"].rearrange("p j s -> p (j s)"),
                          D["ldt_R"].rearrange("p j s -> p (j s)"), [128, NJ * 128], "R")
        br = load(T, "brR", [128, NJ * 128], F32, D["bre_R"].rearrange("p j s -> p (j s)"))
        bi = load(T, "biR", [128, NJ * 128], F32, D["bim_R"].rearrange("p j s -> p (j s)"))
        k = ["R", "brR", "biR"]
        tt(tr_["t1"][:], tr_["fr"][:], br[:], ALU.mult, k, k)
        tt(tr_["t2"][:], tr_["fi"][:], bi[:], ALU.mult, k, k)
        tt(BbT[0][:].rearrange("p j s -> p (j s)"), tr_["t1"][:], tr_["t2"][:], ALU.subtract, k, ["BbT"])
        tt(tr_["t1"][:], tr_["fr"][:], bi[:], ALU.mult, k, k)
        tt(tr_["t2"][:], tr_["fi"][:], br[:], ALU.mult, k, k)
        tt(BbT[1][:].rearrange("p j s -> p (j s)"), tr_["t1"][:], tr_["t2"][:], ALU.add, k, ["BbT"])
        for x_ in range(2):
            for pl in range(3):
                dv(lambda e, x_=x_, pl=pl: e.tensor_copy(out=BbP[x_][32 * pl:32 * pl + 32, :, pl, :], in_=BbT[x_][32 * pl:32 * pl + 32, :, :]), ["BbT", "BbP"], ["BbP"])
        p.fence()
        p.flush()
    Ec = p.sb(P1, "Ec", [128, 32, 128], F32)
    Es = p.sb(P1, "Es", [128, 32, 128], F32)
    Rt = p.sb(P1, "Rt", [128, 32], F32)
    with ExitStack() as T:
        CT = [p.sb(T, "CTr", [128, 32, 32], BF16), p.sb(T, "CTi", [128, 32, 32], BF16)]
        ts_ = cexp_tables(T, D["lre_S"], D["lim_S"], D["ldt_S"], [128, 32], "S")
        k = ["S"]
        dv(lambda e: e.tensor_copy(out=Ec[:, :, 0], in_=ts_["c"][:]), k, ["E"])
        dv(lambda e: e.tensor_copy(out=Es[:, :, 0], in_=ts_["s"][:]), k, ["E"])
        tmpA = p.sb(T, "tmpA", [128, 32, 64], F32)
        tmpB = p.sb(T, "tmpB", [128, 32, 64], F32)
        m = 1
        while m < 128:
            cm = Ec[:, :, m - 1:m].to_broadcast([128, 32, m])
            sm = Es[:, :, m - 1:m].to_broadcast([128, 32, m])
            kk = ["E", "tmp"]
            tt(tmpA[:, :, 0:m], Ec[:, :, 0:m], cm, ALU.mult, kk, kk)
            tt(tmpB[:, :, 0:m], Es[:, :, 0:m], sm, ALU.mult, kk, kk)
            tt(Ec[:, :, m:2 * m], tmpA[:, :, 0:m], tmpB[:, :, 0:m], ALU.subtract, kk, kk)
            tt(tmpA[:, :, 0:m], Ec[:, :, 0:m], sm, ALU.mult, kk, kk)
            tt(tmpB[:, :, 0:m], Es[:, :, 0:m], cm, ALU.mult, kk, kk)
            tt(Es[:, :, m:2 * m], tmpA[:, :, 0:m], tmpB[:, :, 0:m], ALU.add, kk, kk)
            m *= 2
        dv(lambda e: e.tensor_copy(out=Rt[:], in_=ts_["mag"][:]), k, ["Rt"])
        dv(lambda e: e.tensor_copy(out=aS[0][:], in_=ts_["ar"][:]), k, ["aS"])
        dv(lambda e: e.tensor_copy(out=aS[1][:], in_=ts_["ai"][:]), k, ["aS"])
        dv(lambda e: e.tensor_copy(out=fS[0][:], in_=ts_["fr"][:]), k, ["fS"])
        dv(lambda e: e.tensor_copy(out=fS[1][:], in_=ts_["fi"][:]), k, ["fS"])
        cr = load(T, "crS", [128, 32, 32], F32, D["cre_S"])
        ci = load(T, "ciS", [128, 32, 32], F32, D["cim_S"])
        dv(lambda e: e.tensor_copy(out=CT[0][:], in_=cr[:]), ["crS"], ["CT"])
        dv(lambda e: e.tensor_scalar(out=CT[1][:], in0=ci[:], scalar1=-1.0, scalar2=None, op0=ALU.mult), ["ciS"], ["CT"])
        for x_ in range(2):
            for pl in range(3):
                npi = len(range(pl, 32, 3))
                dv(lambda e, x_=x_, pl=pl: e.tensor_copy(out=CTP[x_][:, pl::3, 32 * pl:32 * pl + 32], in_=CT[x_][:, pl::3, :]), ["CT", "CTP"], ["CTP"])
        p.fence()
        p.flush()


    if CTX_FAST:
      with ExitStack() as Q:
        GT = [p.sb(Q, "GTr", [128, 32, 128], BF16), p.sb(Q, "GTi", [128, 32, 128], BF16)]
        am = [p.sb(Q, "amr", [128, 32], F32), p.sb(Q, "ami", [128, 32], F32)]
        BbS = [p.sb(Q, "BbSr", [128, 32, 32], F32), p.sb(Q, "BbSi", [128, 32, 32], F32)]
        w_u0 = load(Q, "w_u0", [128, 8, 1024], BF16, D["w_u0"], "pool")
        qs_tr = p.ps(Q, "qs_tr", [128, 1024], BF16)
        qs_u = p.ps(Q, "qs_u", [128, 2, 512], F32)
        qs_m = [p.ps(Q, "qs_m%d" % i, [128, 2, 512], F32) for i in range(2)]
        Q2 = ExitStack()
        Gr = p.sb(Q2, "Gr", [128, 32, 128], F32)
        Gi = p.sb(Q2, "Gi", [128, 32, 128], F32)
        tq = [p.sb(Q2, "tq%d" % i, [128, 32, 64], F32) for i in range(2)]
        ts3 = [p.sb(Q2, "ts3%d" % i, [128, 32], F32) for i in range(3)]
        bSr = tq[0][:, :, 0:32]
        bSi = tq[0][:, :, 32:64]
        p.dma("sp", bSr, D["bre_S"], writes=["bSr"])
        p.dma("sp", bSi, D["bim_S"], writes=["bSi"])
        t_a = tq[1][:, :, 0:32]
        t_b = tq[1][:, :, 32:64]
        frb = fS[0][:].unsqueeze(2).to_broadcast([128, 32, 32])
        fib = fS[1][:].unsqueeze(2).to_broadcast([128, 32, 32])
        kb_ = ["fS", "bSr", "bSi", "tqb"]
        tt(t_a, bSr, frb, ALU.mult, kb_, ["tqb"])
        tt(t_b, bSi, fib, ALU.mult, kb_, ["tqb"])
        tt(BbS[0][:], t_a, t_b, ALU.subtract, ["tqb"], ["BbS"])
        tt(t_a, bSi, frb, ALU.mult, kb_ + ["BbS"], ["tqb"])
        tt(t_b, bSr, fib, ALU.mult, kb_, ["tqb"])
        tt(BbS[1][:], t_a, t_b, ALU.add, ["tqb"], ["BbS"])
        kg = ["G", "tqb", "bSr", "bSi"]
        dv(lambda e: e.memset(Gr[:, :, 127:128], 1.0), kg, kg)
        dv(lambda e: e.memset(Gi[:, :, 127:128], 0.0), kg, kg)
        dv(lambda e: e.tensor_copy(out=am[0][:], in_=aS[0][:]), ["aS"], kg)
        dv(lambda e: e.tensor_copy(out=am[1][:], in_=aS[1][:]), ["aS"], kg)
        m = 1
        while m < 128:
            src_r = Gr[:, :, 128 - m:128]
            src_i = Gi[:, :, 128 - m:128]
            amr = am[0][:].unsqueeze(2).to_broadcast([128, 32, m])
            ami = am[1][:].unsqueeze(2).to_broadcast([128, 32, m])
            tt(tq[0][:, :, 0:m], src_r, amr, ALU.mult, kg, kg)
            tt(tq[1][:, :, 0:m], src_i, ami, ALU.mult, kg, kg)
            tt(Gr[:, :, 128 - 2 * m:128 - m], tq[0][:, :, 0:m], tq[1][:, :, 0:m], ALU.subtract, kg, kg)
            tt(tq[0][:, :, 0:m], src_r, ami, ALU.mult, kg, kg)
            tt(tq[1][:, :, 0:m], src_i, amr, ALU.mult, kg, kg)
            tt(Gi[:, :, 128 - 2 * m:128 - m], tq[0][:, :, 0:m], tq[1][:, :, 0:m], ALU.add, kg, kg)
            tt(ts3[0][:], am[0][:], am[0][:], ALU.mult, kg, kg)
            tt(ts3[1][:], am[1][:], am[1][:], ALU.mult, kg, kg)
            tt(ts3[2][:], am[0][:], am[1][:], ALU.mult, kg, kg)
            tt(am[0][:], ts3[0][:], ts3[1][:], ALU.subtract, kg, kg)
            dv(lambda e: e.tensor_scalar(out=am[1][:], in0=ts3[2][:], scalar1=2.0, scalar2=None, op0=ALU.mult), kg, kg)
            m *= 2
        for x_, Gsrc in enumerate((Gr, Gi)):
            for q4 in range(8):
                pst = qs_m[q4 % 2]
                for i4 in range(4):
                    pi = q4 * 4 + i4
                    p.op("pe", lambda e, pi=pi, i4=i4, Gsrc=Gsrc, pst=pst: e.transpose(out=pst[:, i4 // 4, (i4 % 4) * 128:(i4 % 4 + 1) * 128], in_=Gsrc[:, pi, :], identity=identf[:]),
                         reads=["G", "identf"], writes=["qs_m%d" % (q4 % 2)])
                p.op("act", lambda e, x_=x_, q4=q4, pst=pst: e.activation(out=GT[x_][:, q4 * 4:q4 * 4 + 4, :].rearrange("p a s -> p (a s)"), in_=pst[:, 0, :], func=AF.Copy),
                     reads=["qs_m%d" % (q4 % 2)], writes=["GT"])
        p.fence()
        p.flush()
        Q2.close()
        xa = [p.sb(Q, "xa%d" % i, [128, 1024], F32) for i in range(2)]
        junka = p.sb(Q, "junka", [128, 1024], BF16)
        ssa = [p.sb(Q, "ssa%d" % i, [128, 1], F32) for i in range(2)]
        rstda = [p.sb(Q, "rstda%d" % i, [128, 1], F32) for i in range(2)]
        xsa = [p.sb(Q, "xsa%d" % i, [128, 1024], BF16) for i in range(2)]
        hna = [p.sb(Q, "hna%d" % i, [128, 8, 128], BF16) for i in range(2)]
        utk = [p.sb(Q, "utk%d" % i, [128, 1024], BF16) for i in range(2)]
        Ms = [[p.sb(Q, "Ms%d%d" % (x_, i), [128, 32, 32], F32) for i in range(2)] for x_ in range(2)]
        pw = [p.sb(Q, "pw%d" % i, [128, 32, 32], F32) for i in range(4)]
        Vt = [p.sb(Q, "Vt%d" % x_, [128, 32], F32) for x_ in range(2)]
        t8 = [p.sb(Q, "t8%d" % i, [128, 32], F32) for i in range(4)]

        def ctx_pro1(lb):
            bb = lb % 2
            sx = "%d" % bb
            p.dma("sp", xa[bb][:], D["xloc"][lb * 128:(lb + 1) * 128, :], writes=["xa" + sx])
            p.op("act", lambda e, bb=bb: e.activation(out=junka[:], in_=xa[bb][:], func=AF.Square, accum_out=ssa[bb][:]), reads=["xa" + sx], writes=["junka", "ssa" + sx])
            p.op("act", lambda e, bb=bb: e.activation(out=rstda[bb][:], in_=ssa[bb][:], func=AF.Sqrt, scale=1.0 / 1024, bias=EPS), reads=["ssa" + sx], writes=["rstda" + sx])
            dv(lambda e, bb=bb: e.reciprocal(out=rstda[bb][:], in_=rstda[bb][:]), ["rstda" + sx], ["rstda" + sx])
            p.op("act", lambda e, bb=bb: e.activation(out=xsa[bb][:], in_=xa[bb][:], func=AF.Copy, scale=rstda[bb][:, 0:1]), reads=["xa" + sx, "rstda" + sx], writes=["xsa" + sx])
            for kc in range(8):
                p.op("pe", lambda e, kc=kc, bb=bb: e.transpose(out=qs_tr[:, kc * 128:(kc + 1) * 128], in_=xsa[bb][:, kc * 128:(kc + 1) * 128], identity=ident[:]),
                     reads=["xsa" + sx, "ident"], writes=["qs_tr"])

        def ctx_pro2(lb):
            bb = lb % 2
            sx = "%d" % bb
            tt(hna[bb][:], qs_tr[:].rearrange("p (k t) -> p k t", k=8), g_mix[:].unsqueeze(2).to_broadcast([128, 8, 128]), ALU.mult,
               ["qs_tr", "g_mix"], ["hna" + sx])
            for hf in range(2):
                for kc in range(8):
                    p.op("pe", lambda e, hf=hf, kc=kc, bb=bb: e.matmul(qs_u[:, hf, :], lhsT=hna[bb][:, kc, :], rhs=w_u0[:, kc, hf * 512:(hf + 1) * 512],
                                                                       start=(kc == 0), stop=(kc == 7)), reads=["w_u0", "hna" + sx], writes=["qs_u"])
            p.op("act", lambda e, bb=bb: e.activation(out=utk[bb][:], in_=qs_u[:].rearrange("p a t -> p (a t)"), func=AF.Copy), reads=["qs_u"], writes=["utk" + sx])

        def ctx_pro3(lb):
            bb = lb % 2
            sx = "%d" % bb
            for x_ in range(2):
                for pi in range(32):
                    p.op("pe", lambda e, x_=x_, pi=pi, bb=bb: e.matmul(qs_m[x_][:, pi // 16, (pi % 16) * 32:(pi % 16) * 32 + 32], lhsT=GT[x_][:, pi, :],
                                                                       rhs=utk[bb][:, 32 * pi:32 * pi + 32], start=True, stop=True),
                         reads=["GT", "utk" + sx], writes=["qs_m%d" % x_])
                p.op("act", lambda e, x_=x_, bb=bb: e.activation(out=Ms[x_][bb][:].rearrange("p a c -> p (a c)"), in_=qs_m[x_][:].rearrange("p a t -> p (a t)"), func=AF.Copy),
                     reads=["qs_m%d" % x_], writes=["Ms%d" % x_ + sx])

        for i0 in range(0, NE * CAP + 128, 128):
            p.dma("pool", Xs_d[i0:i0 + 128, :], zrow[:], reads=["zrow"], writes=["Xs_d"], stream="xsz")
        ctx_pro1(0)
        ctx_pro2(0)
        ctx_pro3(0)
        for lb in range(NB_CTX - 1):
            bb = lb % 2
            sx = "%d" % bb
            if lb + 1 < NB_CTX - 1:
                ctx_pro1(lb + 1)
                ctx_pro2(lb + 1)
                ctx_pro3(lb + 1)
            Mr, Mi = Ms[0][bb], Ms[1][bb]
            kr, ki = "Ms0" + sx, "Ms1" + sx
            tt(pw[0][:], Mr[:], BbS[0][:], ALU.mult, [kr, "BbS"], ["pw0"])
            tt(pw[1][:], Mi[:], BbS[1][:], ALU.mult, [ki, "BbS"], ["pw1"])
            tt(pw[2][:], Mi[:], BbS[0][:], ALU.mult, [ki, "BbS"], ["pw2"])
            tt(pw[3][:], Mr[:], BbS[1][:], ALU.mult, [kr, "BbS"], ["pw3"])
            tt(pw[0][:], pw[0][:], pw[1][:], ALU.subtract, ["pw0", "pw1"], ["pw0"])
            tt(pw[2][:], pw[2][:], pw[3][:], ALU.add, ["pw2", "pw3"], ["pw2"])
            dv(lambda e: e.reduce_sum(out=Vt[0][:], in_=pw[0][:], axis=AX.X), ["pw0"], ["Vt0"])
            dv(lambda e: e.reduce_sum(out=Vt[1][:], in_=pw[2][:], axis=AX.X), ["pw2"], ["Vt1"])
            tt(t8[0][:], am[0][:], Xc[0][:], ALU.mult, ["G", "Xc"], ["t80"])
            tt(t8[1][:], am[1][:], Xc[1][:], ALU.mult, ["G", "Xc"], ["t81"])
            tt(t8[2][:], am[0][:], Xc[1][:], ALU.mult, ["G", "Xc"], ["t82"])
            tt(t8[3][:], am[1][:], Xc[0][:], ALU.mult, ["G", "Xc"], ["t83"])
            tt(t8[0][:], t8[0][:], t8[1][:], ALU.subtract, ["t80", "t81"], ["t80"])
            tt(t8[2][:], t8[2][:], t8[3][:], ALU.add, ["t82", "t83"], ["t82"])
            tt(Xc[0][:], t8[0][:], Vt[0][:], ALU.add, ["t80", "Vt0", "Xc"], ["Xc"])
            tt(Xc[1][:], t8[2][:], Vt[1][:], ALU.add, ["t82", "Vt1", "Xc"], ["Xc"])
        p.fence()
        p.flush()

    if stop == "tables":
        p.fence(); p.flush(); return nc
    xin = [p.sb(P1, "xin0", [128, 1024], F32)] * 2
    junk = p.sb(P1, "junk", [128, 1024], BF16)
    ss = p.sb(P1, "ss", [128, 1], F32)
    rstd = p.sb(P1, "rstd", [128, 1], F32)
    xsL = [p.sb(P1, "xs0", [128, 1024], BF16)] * 2
    hnL = [p.sb(P1, "hnT%d" % i, [128, 8, 128], BF16) for i in range(2)]
    uTbL = [p.sb(P1, "uTb%d" % i, [128, NJ, 128], BF16) for i in range(2)]
    uTfL = uTbL
    wk = [p.sb(P1, "wk%d" % i, [128, 8, 128], F32) for i in range(4)]
    xr = [p.sb(P1, "xr_r", [128, 32, 128], BF16), p.sb(P1, "xr_i", [128, 32, 128], BF16)]
    ysb = p.sb(P1, "ysb", [128, NJ, 128], F32)
    zTb = p.sb(P1, "zTb", [128, NJ, 128], BF16)
    qkf = p.sb(P1, "qkf", [128, 18, 64], F32)
    rc = p.sb(P1, "rc", [128, 8], F32)
    rs = p.sb(P1, "rs", [128, 8], F32)
    rt = [p.sb(P1, "rt%d" % i, [128, 18, 8], F32) for i in range(4)]
    qkb = p.sb(P1, "qkb", [128, 18, 64], BF16)
    qT = p.sb(P1, "qT", [64, 16, 128], BF16)
    kT = [p.sb(P1, "kT%d" % i, [64, 2, 128], BF16) for i in range(3)]
    vA = [p.sb(P1, "vA%d" % i, [128, 2, 65], BF16) for i in range(3)]
    for i in range(3):
        p.op("pool", lambda e, i=i: e.memset(vA[i][:], 1.0), writes=["vA%d" % i])
    Et = [p.sb(P1, "Et%d" % i, [128, 512], BF16) for i in range(4)]
    Etm = [p.sb(P1, "Etm%d" % i, [128, 512], BF16) for i in range(4)]
    Em = [p.sb(P1, "Em%d" % i, [16, 512], BF16) for i in range(4)]
    den = p.sb(P1, "den", [128, 16], F32)
    attn = p.sb(P1, "attn", [128, 16, 64], BF16)
    aT = p.sb(P1, "aT", [128, 8, 128], BF16)

    ps_tr = p.ps(P1, "ps_tr", [128, 1024], BF16)
    ps_u = p.ps(P1, "ps_u", [128, 3, 512], F32)
    ps_b = [p.ps(P1, "ps_b%d" % i, [128, 2, 512], F32) for i in range(2)]

    def norm_block(src, par):
        xt = xin[par]
        xk = "xin0"
        xs = xsL[par]
        hnT = hnL[par]
        kxs = "xs0"
        khn = "hnT%d" % par
        p.dma("sp", xt[:], src, writes=[xk])
        p.op("act", lambda e: e.activation(out=junk[:], in_=xt[:], func=AF.Square, accum_out=ss[:]), reads=[xk], writes=["junk", "ss"])
        p.op("act", lambda e: e.activation(out=rstd[:], in_=ss[:], func=AF.Sqrt, scale=1.0 / 1024, bias=EPS), reads=["ss"], writes=["rstd"])
        dv(lambda e: e.reciprocal(out=rstd[:], in_=rstd[:]), ["rstd"], ["rstd"])
        dv(lambda e: e.tensor_scalar(out=xs[:], in0=xt[:], scalar1=rstd[:, 0:1], scalar2=None, op0=ALU.mult), [xk, "rstd"], [kxs])
        for kc in range(8):
            p.op("pe", lambda e, kc=kc: e.transpose(out=ps_tr[:, kc * 128:(kc + 1) * 128], in_=xs[:, kc * 128:(kc + 1) * 128], identity=ident[:]),
                 reads=[kxs, "ident"], writes=["ps_tr"])
        tt(hnT[:], ps_tr[:].rearrange("p (k t) -> p k t", k=8), g_mix[:].unsqueeze(2).to_broadcast([128, 8, 128]), ALU.mult,
           ["ps_tr", "g_mix"], [khn])

    def uproj(par):
        hnT = hnL[par]
        uTf = uTfL[par]
        uTb = uTbL[par]
        khn = "hnT%d" % par
        kuf = "uTb%d" % par
        kub = "uTb%d" % par
        for j in range(NJ):
            for kc in range(8):
                p.op("pe", lambda e, j=j, kc=kc: e.matmul(ps_u[:, j // 4, (j % 4) * 128:(j % 4 + 1) * 128], lhsT=w_u[:, kc, j * 128:(j + 1) * 128],
                                                          rhs=hnT[:, kc, :], start=(kc == 0), stop=(kc == 7)),
                     reads=["w_u", khn], writes=["ps_u"])
        for b3 in range(3):
            n = 4 if b3 < 2 else 3
            p.op("act", lambda e, b3=b3, n=n: e.activation(out=uTb[:, b3 * 4:b3 * 4 + n, :], in_=ps_u[:, b3, 0:n * 128].rearrange("p (j t) -> p j t", j=n), func=AF.Copy),
                 reads=["ps_u"], writes=[kub])

    W127 = [p.sb(P1, "W127r", [128, 32], F32), p.sb(P1, "W127i", [128, 32], F32)]
    c6 = [p.sb(P1, "c6%d" % i, [128, 32], F32) for i in range(4)]

    def ssm_block(own, par):
        A, B, Cc, Dd = wk
        uTf = uTfL[par]
        uTb = uTbL[par]
        kuf = "uTb%d" % par
        kub = "uTb%d" % par
        for g8 in range(4):
            pb = ps_b
            for x_ in range(2):
                for pl8 in range(8):
                    pi = g8 * 8 + pl8
                    j, pl = divmod(pi, 3)
                    p.op("pe", lambda e, x_=x_, pl8=pl8, j=j, pl=pl: e.matmul(
                        pb[x_][:, pl8 // 4, (pl8 % 4) * 128:(pl8 % 4 + 1) * 128], lhsT=BbP[x_][:, j, pl, :],
                        rhs=uTb[:, j, :], start=True, stop=True), reads=["BbP", kub], writes=["ps_b%d" % x_])
            sl = slice(g8 * 8, g8 * 8 + 8)
            kA = [("wA", i) for i in range(8)]
            kB = [("wB", i) for i in range(8)]
            kC = [("wC", i) for i in range(8)]
            kD = [("wD", i) for i in range(8)]
            for hb in range(2):
                s4 = slice(g8 * 8 + hb * 4, g8 * 8 + hb * 4 + 4)
                h4 = slice(hb * 4, hb * 4 + 4)
                k4 = slice(hb * 4, hb * 4 + 4)
                br4 = pb[0][:, hb, :].rearrange("p (b t) -> p b t", b=4)
                bi4 = pb[1][:, hb, :].rearrange("p (b t) -> p b t", b=4)
                tt(A[:, h4, :], br4, Ec[:, s4, :], ALU.mult, ["E", "ps_b0"], kA[k4])
                tt(Cc[:, h4, :], bi4, Es[:, s4, :], ALU.mult, ["E", "ps_b1"], kC[k4])
                tt(B[:, h4, :], bi4, Ec[:, s4, :], ALU.mult, ["E", "ps_b1"], kB[k4])
                tt(Dd[:, h4, :], br4, Es[:, s4, :], ALU.mult, ["E", "ps_b0"], kD[k4])
                tt(A[:, h4, :], A[:, h4, :], Cc[:, h4, :], ALU.add, kA[k4] + kC[k4], kA[k4])
                tt(B[:, h4, :], B[:, h4, :], Dd[:, h4, :], ALU.subtract, kB[k4] + kD[k4], kB[k4])
            for pl8 in range(8):
                pi = g8 * 8 + pl8
                dv(lambda e, pl8=pl8, pi=pi: e.tensor_tensor_scan(out=Cc[:, pl8, :], data0=Rt[:, pi:pi + 1].to_broadcast([128, 128]), data1=A[:, pl8, :],
                                                                  initial=Xc[0][:, pi:pi + 1], op0=ALU.mult, op1=ALU.add), [kA[pl8], "Rt", "Xc"], [kC[pl8]])
            for pl8 in range(8):
                pi = g8 * 8 + pl8
                dv(lambda e, pl8=pl8, pi=pi: e.tensor_tensor_scan(out=Dd[:, pl8, :], data0=Rt[:, pi:pi + 1].to_broadcast([128, 128]), data1=B[:, pl8, :],
                                                                  initial=Xc[1][:, pi:pi + 1], op0=ALU.mult, op1=ALU.add), [kB[pl8], "Rt", "Xc"], [kD[pl8]])
            if own:
                tt(A[:], Ec[:, sl, :], Cc[:], ALU.mult, kC + kA + ["E"], kA)
                tt(B[:], Es[:, sl, :], Dd[:], ALU.mult, kD + kB + ["E"], kB)
                tt(xr[0][:, sl, :], A[:], B[:], ALU.subtract, kA + kB, [("xr0", g8)])
            dv(lambda e, sl=sl: e.tensor_copy(out=W127[0][:, sl], in_=Cc[:, :, 127]), kC, ["W127r"])
            dv(lambda e, sl=sl: e.tensor_copy(out=W127[1][:, sl], in_=Dd[:, :, 127]), kD, ["W127i"])
            if own:
                tt(A[:], Ec[:, sl, :], Dd[:], ALU.mult, kD + kA + ["E"], kA)
                tt(B[:], Es[:, sl, :], Cc[:], ALU.mult, kC + kB + ["E"], kB)
                tt(xr[1][:, sl, :], A[:], B[:], ALU.add, kA + kB, [("xr1", g8)])
            yield
        e_c = Ec[:, :, 127]
        e_s = Es[:, :, 127]
        tt(c6[0][:], e_c, W127[0][:], ALU.mult, ["E", "W127r"], ["c60"])
        tt(c6[1][:], e_s, W127[1][:], ALU.mult, ["E", "W127i"], ["c61"])
        tt(c6[2][:], e_c, W127[1][:], ALU.mult, ["E", "W127i"], ["c62"])
        tt(c6[3][:], e_s, W127[0][:], ALU.mult, ["E", "W127r"], ["c63"])
        tt(Xc[0][:], c6[0][:], c6[1][:], ALU.subtract, ["c60", "c61", "Xc"], ["Xc"])
        tt(Xc[1][:], c6[2][:], c6[3][:], ALU.add, ["c62", "c63", "Xc"], ["Xc"])
        if own:
            kx = [("xr0", g) for g in range(4)] + [("xr1", g) for g in range(4)]
            for pi in range(32):
                j, pl = divmod(pi, 3)
                lastpl = 2 if j < NJ - 1 else 1
                for x_ in range(2):
                    p.op("pe", lambda e, pi=pi, j=j, pl=pl, x_=x_, lastpl=lastpl: e.matmul(
                        ps_u[:, j // 4, (j % 4) * 128:(j % 4 + 1) * 128], lhsT=CTP[x_][:, pi, :], rhs=xr[x_][:, pi, :],
                        start=(pl == 0 and x_ == 0), stop=(pl == lastpl and x_ == 1)), reads=["CTP", ("xr%d" % x_, pi // 8)], writes=["ps_u"])
            tt(ysb[:], uTf[:], d_pad[:].unsqueeze(2).to_broadcast([128, NJ, 128]), ALU.mult, [kuf, "d_pad"], ["ysb"])
            for b3 in range(3):
                n = 4 if b3 < 2 else 3
                tt(ysb[:, b3 * 4:b3 * 4 + n, :], ysb[:, b3 * 4:b3 * 4 + n, :], ps_u[:, b3, 0:n * 128].rearrange("p (j t) -> p j t", j=n), ALU.add, ["ysb", "ps_u"], ["ysb"])
            p.op("act", lambda e: e.activation(out=zTb[:], in_=ysb[:], func=AF.Gelu), reads=["ysb"], writes=["zTb"])

    def qkv_block(bidx, slot, par, meta=False):
        hnT = hnL[par]
        khn = "hnT%d" % par
        for c0, c1 in ((0, 512), (512, 1024), (1024, 1280)):
            for kc in range(8):
                p.op("pe", lambda e, c0=c0, c1=c1, kc=kc: e.matmul(ps_u[:, c0 // 512, 0:c1 - c0], lhsT=hnT[:, kc, :], rhs=w_qkv[:, kc, c0:c1],
                                                                   start=(kc == 0), stop=(kc == 7)), reads=[khn, "w_qkv"], writes=["ps_u"])
        p.dma("sp", rc[:], D["rcos"][bidx], writes=["rc"])
        p.dma("sp", rs[:], D["rsin"][bidx], writes=["rs"])
        p.op("act", lambda e: e.activation(out=qkf[:, 0:16, :].rearrange("p h d -> p (h d)"), in_=ps_u[:, 0:2, :].rearrange("p a b -> p (a b)"), func=AF.Copy),
             reads=["ps_u"], writes=["qkf"])
        p.op("act", lambda e: e.activation(out=qkf[:, 16:18, :].rearrange("p h d -> p (h d)"), in_=ps_u[:, 2, 0:128], func=AF.Copy),
             reads=["ps_u"], writes=["qkf"])
        vk = "vA%d" % slot
        p.op("act", lambda e: e.activation(out=vA[slot][:, :, 0:64], in_=ps_u[:, 2, 128:256].rearrange("p (h d) -> p h d", h=2), func=AF.Copy),
             reads=["ps_u"], writes=[vk])
        p.op("act", lambda e: e.activation(out=qkb[:].rearrange("p h d -> p (h d)"), in_=qkf[:].rearrange("p h d -> p (h d)"), func=AF.Copy), reads=["qkf"], writes=["qkb"])
        cb = rc[:].unsqueeze(1).to_broadcast([128, 18, 8])
        sbb = rs[:].unsqueeze(1).to_broadcast([128, 18, 8])
        x1 = qkf[:, :, 0:8]
        x2 = qkf[:, :, 8:16]
        kr = ["qkf", "rc", "rs", "rt"]
        tt(rt[0][:], x1, cb, ALU.mult, kr, ["rt"])
        tt(rt[1][:], x2, sbb, ALU.mult, kr, ["rt"])
        tt(qkb[:, :, 0:8], rt[0][:], rt[1][:], ALU.subtract, ["rt", "qkb"], ["qkb"])
        tt(rt[2][:], x2, cb, ALU.mult, kr, ["rt"])
        tt(rt[3][:], x1, sbb, ALU.mult, kr, ["rt"])
        tt(qkb[:, :, 8:16], rt[2][:], rt[3][:], ALU.add, ["rt", "qkb"], ["qkb"])
        kk = "kT%d" % slot
        for h in range(2):
            p.op("pe", lambda e, h=h: e.transpose(out=ps_tr[0:64, h * 128:(h + 1) * 128], in_=qkb[:, 16 + h, :], identity=ident[:]),
                 reads=["qkb", "ident"], writes=["ps_tr"])
        dv(lambda e: e.tensor_copy(out=kT[slot][:].rearrange("p h t -> p (h t)"), in_=ps_tr[0:64, 0:256]), ["ps_tr"], [kk])
        if not meta:
            for half in range(2):
                for h in range(8):
                    p.op("pe", lambda e, h=h, half=half: e.transpose(out=ps_tr[0:64, h * 128:(h + 1) * 128], in_=qkb[:, half * 8 + h, :], identity=ident[:]),
                         reads=["qkb", "ident"], writes=["ps_tr"])
                dv(lambda e, half=half: e.tensor_copy(out=qT[:, half * 8:half * 8 + 8, :].rearrange("p h t -> p (h t)"), in_=ps_tr[0:64, :]), ["ps_tr"], ["qT"])

    scale = 1.0 / math.sqrt(64.0)

    def attn_block(ob, cur, prev, first):
        mp = m_first if first else m_prev
        for kap in range(2):
            for half in range(2):
                rhs = qT[:, kap * 8 + half * 4:kap * 8 + half * 4 + 4, :].rearrange("p h t -> p (h t)")
                i = kap * 2 + half
                pbank = ps_b[half]
                p.op("pe", lambda e, kap=kap, rhs=rhs, pbank=pbank: e.matmul(pbank[:, 0, :], lhsT=kT[prev][:, kap, :], rhs=rhs, start=True, stop=True),
                     reads=["kT%d" % prev, "qT"], writes=["ps_b%d" % half])
                p.op("pe", lambda e, kap=kap, rhs=rhs, pbank=pbank: e.matmul(pbank[:, 1, :], lhsT=kT[cur][:, kap, :], rhs=rhs, start=True, stop=True),
                     reads=["kT%d" % cur, "qT"], writes=["ps_b%d" % half])
                ek = "Et%d" % i
                p.op("act", lambda e, i=i, pbank=pbank: e.activation(out=Et[i][:], in_=pbank[:, 0, :], func=AF.Exp, scale=scale), reads=["ps_b%d" % half], writes=[ek])
                p.op("act", lambda e, i=i, pbank=pbank: e.activation(out=Etm[i][:], in_=pbank[:, 1, :], func=AF.Exp, scale=scale), reads=["ps_b%d" % half], writes=[ek + "c"])
                tt(Et[i][:].rearrange("p (h t) -> p h t", h=4), Et[i][:].rearrange("p (h t) -> p h t", h=4), mp[:].unsqueeze(1).to_broadcast([128, 4, 128]),
                   ALU.mult, [ek, "m_first", "m_prev"], [ek])
                tt(Etm[i][:].rearrange("p (h t) -> p h t", h=4), Etm[i][:].rearrange("p (h t) -> p h t", h=4), m_cur[:].unsqueeze(1).to_broadcast([128, 4, 128]),
                   ALU.mult, [ek + "c", "m_cur"], [ek + "c"])
                p.op("pe", lambda e, kap=kap, rhs=rhs: e.matmul(ps_u[0:16, 2, :], lhsT=kT[2][:, kap, 0:16], rhs=rhs, start=True, stop=True),
                     reads=["kT2", "qT"], writes=["ps_u"])
                p.op("act", lambda e, i=i: e.activation(out=Em[i][:], in_=ps_u[0:16, 2, :], func=AF.Exp, scale=scale), reads=["ps_u"], writes=["Em%d" % i])
            yield
        for h in range(16):
            kap = h // 8
            i = kap * 2 + (h % 8) // 4
            c = (h % 4) * 128
            o = ps_u[:, h // 7, (h % 7) * 65:(h % 7) * 65 + 65]
            p.op("pe", lambda e, i=i, c=c, o=o, kap=kap: e.matmul(o, lhsT=Et[i][:, c:c + 128], rhs=vA[prev][:, kap, :], start=True, stop=False),
                 reads=["Et%d" % i, "vA%d" % prev], writes=["ps_u"])
            p.op("pe", lambda e, i=i, c=c, o=o, kap=kap: e.matmul(o, lhsT=Etm[i][:, c:c + 128], rhs=vA[cur][:, kap, :], start=False, stop=False),
                 reads=["Et%dc" % i, "vA%d" % cur], writes=["ps_u"])
            p.op("pe", lambda e, i=i, c=c, o=o, kap=kap: e.matmul(o, lhsT=Em[i][:, c:c + 128], rhs=vA[2][0:16, kap, :], start=False, stop=True),
                 reads=["Em%d" % i, "vA2"], writes=["ps_u"])
        for bk in range(3):
            n = 7 if bk < 2 else 2
            h0 = bk * 7
            pv = ps_u[:, bk, 0:n * 65].rearrange("p (h d) -> p h d", h=n)
            tt(den[:, h0:h0 + n], pv[:, :, 64], esink[:, h0:h0 + n], ALU.add, ["ps_u", "esink"], ["den"])
            dv(lambda e, h0=h0, n=n: e.reciprocal(out=den[:, h0:h0 + n], in_=den[:, h0:h0 + n]), ["den"], ["den"])
            tt(attn[:, h0:h0 + n, :], pv[:, :, 0:64], den[:, h0:h0 + n].unsqueeze(2).to_broadcast([128, n, 64]), ALU.mult, ["ps_u", "den"], ["attn"])
        yield
        for kc in range(8):
            p.op("pe", lambda e, kc=kc: e.transpose(out=ps_tr[:, kc * 128:(kc + 1) * 128], in_=attn[:, 2 * kc:2 * kc + 2, :].rearrange("p h d -> p (h d)"), identity=ident[:]),
                 reads=["attn", "ident"], writes=["ps_tr"])
        dv(lambda e: e.tensor_copy(out=aT[:].rearrange("p k t -> p (k t)"), in_=ps_tr[:]), ["ps_tr"], ["aT"])
        p.dma("sp", aT_d[ob], aT[:].rearrange("p k t -> p (k t)"), reads=["aT"], writes=["aT_d"])
        yield


    norm_block(D["xmeta"], 1)
    qkv_block(0, 2, 1, meta=True)
    if stop == "meta":
        p.fence(); p.flush(); return nc
    blocks = list(range(NB_CTX - 1 if CTX_FAST else 0, NB))

    def pro_a(lb, par):
        norm_block(D["xloc"][lb * 128:(lb + 1) * 128, :], par)
        uproj(par)

    def pro_b(lb, par):
        if lb >= NB_CTX - 1:
            qkv_block(lb + 1, lb % 2, par, meta=False)
    pro_a(blocks[0], 0)
    pro_b(blocks[0], 0)
    for bi, lb in enumerate(blocks):
        par = bi % 2
        own = lb >= NB_CTX
        nxt = blocks[bi + 1] if bi + 1 < len(blocks) else None
        gs = ssm_block(own, par)
        ga = attn_block(lb - NB_CTX, lb % 2, (lb - 1) % 2, lb == NB_CTX) if own else iter(())
        for st in range(4):
            next(gs)
            next(ga, None)
            if st == 0 and nxt is not None:
                pro_a(nxt, 1 - par)
        for _ in gs:
            pass
        for _ in ga:
            pass
        if own:
            ob = lb - NB_CTX
            p.dma("sp", zT_d[ob], zTb[:].rearrange("p j t -> p (j t)"), reads=["zTb"], writes=["zT_d"])
            p.dma("sp", hT_d[ob], hnL[par][:].rearrange("p k t -> p (k t)"), reads=["hnT%d" % par], writes=["hT_d"])
        if nxt is not None:
            pro_b(nxt, 1 - par)
    p.fence()
    p.flush()
    if stop == "p1":
        return nc
    P1.close()

    P2 = ExitStack()
    w_g = load(P2, "w_g", [128, 8, 2048], BF16, D["w_g"], "pool")
    w_glu = load(P2, "w_glu", [128, NJ, NJ * 128], BF16, D["w_glu"], "pool")
    w_brs = load(P2, "w_brs", [128, NJ, 1024], BF16, D["w_brs"], "pool")
    w_bra = load(P2, "w_bra", [128, 8, 1024], BF16, D["w_bra"], "pool")
    w_out = load(P2, "w_out", [128, 8, 1024], BF16, D["w_out"], "pool")
    w_rt = load(P2, "w_rt", [128, 8, 32], F32, D["w_rt"])
    b_glu = load(P2, "b_glu", [128, NJ], F32, D["b_glu"])
    b_rt = load(P2, "b_rt", [128, 32], F32, D["b_rt"])
    g_ffn = load(P2, "g_ffn", [128, 1024], F32, D["g_ffn"])
    tri = load(P2, "tri", [128, 128], BF16, D["tri"], "pool")
    ones = p.sb(P2, "ones", [128, 128], BF16)
    p.op("pool", lambda e: e.memset(ones[:], 1.0), writes=["ones"])
    iota_i = p.sb(P2, "iota_i", [128, 32], I32)
    iota_f = p.sb(P2, "iota_f", [128, 32], F32)
    p.op("pool", lambda e: e.iota(iota_i[:], pattern=[[1, 32]], base=0, channel_multiplier=0), writes=["iota_i"])
    dv(lambda e: e.tensor_copy(out=iota_f[:], in_=iota_i[:]), ["iota_i"], ["iota_f"])
    base = p.sb(P2, "base", [128, 32], F32)
    dv(lambda e: e.memset(base[:], 0.0), [], ["base"])

    hT2 = p.sb(P2, "hT2", [128, 8, 128], BF16)
    zT2 = p.sb(P2, "zT2", [128, NJ, 128], BF16)
    aT2 = p.sb(P2, "aT2", [128, 8, 128], BF16)
    x2 = p.sb(P2, "x2", [128, 1024], F32)
    sg = p.sb(P2, "sg", [128, 16, 128], BF16)
    sig = p.sb(P2, "sig", [128, NJ, 128], BF16)
    so = p.sb(P2, "so", [128, NJ, 128], BF16)
    mixa = p.sb(P2, "mixa", [128, 8, 128], F32)
    mixb = p.sb(P2, "mixb", [128, 8, 128], F32)
    mixT = p.sb(P2, "mixT", [128, 8, 128], BF16)
    h2 = p.sb(P2, "h2", [128, 1024], F32)
    junk2 = p.sb(P2, "junk2", [128, 1024], F32)
    ss2 = p.sb(P2, "ss2", [128, 1], F32)
    rstd2 = p.sb(P2, "rstd2", [128, 1], F32)
    xn = p.sb(P2, "xn", [128, 1024], F32)
    xnb = p.sb(P2, "xnb", [128, 1024], BF16)
    xnT = p.sb(P2, "xnT", [128, 8, 128], F32)
    lg = p.sb(P2, "lg", [128, 32], F32)
    top8 = p.sb(P2, "top8", [128, 8], F32)
    idx8 = p.sb(P2, "idx8", [128, 8], U32)
    ex4 = p.sb(P2, "ex4", [128, 4], F32)
    nmx = p.sb(P2, "nmx", [128, 1], F32)
    sm4 = p.sb(P2, "sm4", [128, 1], F32)
    Mb = p.sb(P2, "Mb", [128, 32], BF16)
    pos = p.sb(P2, "pos", [128, 32], F32)
    oh = p.sb(P2, "oh", [128, 32], F32)
    sp4 = p.sb(P2, "sp4", [128, 4], F32)
    slf = p.sb(P2, "slf", [128, 4], F32)
    pq = [p.ps(P2, "pq%d" % i, [128, 512], F32) for i in range(8)]

    for ob in range(NB_OWN):
        p.dma("sp", hT2[:].rearrange("p k t -> p (k t)"), hT_d[ob], reads=["hT_d"], writes=["hT2"])
        p.dma("sp", zT2[:].rearrange("p j t -> p (j t)"), zT_d[ob], reads=["zT_d"], writes=["zT2"])
        p.dma("sp", aT2[:].rearrange("p k t -> p (k t)"), aT_d[ob], reads=["aT_d"], writes=["aT2"])
        p.dma("sp", x2[:], D["xloc"][(NB_CTX + ob) * 128:(NB_CTX + ob + 1) * 128, :], writes=["x2"])
        for oc in range(16):
            pt = pq[oc // 4]
            for kc in range(8):
                p.op("pe", lambda e, oc=oc, kc=kc, pt=pt: e.matmul(pt[:, (oc % 4) * 128:(oc % 4 + 1) * 128], lhsT=w_g[:, kc, oc * 128:(oc + 1) * 128], rhs=hT2[:, kc, :],
                                                                   start=(kc == 0), stop=(kc == 7)), reads=["w_g", "hT2"], writes=["pq%d" % (oc // 4)])
        for b4 in range(4):
            p.op("act", lambda e, b4=b4: e.activation(out=sg[:, b4 * 4:b4 * 4 + 4, :].rearrange("p a t -> p (a t)"), in_=pq[b4][:], func=AF.Sigmoid),
                 reads=["pq%d" % b4], writes=["sg"])
        for oc in range(NJ):
            pt = pq[4 + oc // 4]
            for kc in range(NJ):
                p.op("pe", lambda e, oc=oc, kc=kc, pt=pt: e.matmul(pt[:, (oc % 4) * 128:(oc % 4 + 1) * 128], lhsT=w_glu[:, kc, oc * 128:(oc + 1) * 128], rhs=zT2[:, kc, :],
                                                                   start=(kc == 0), stop=(kc == NJ - 1)), reads=["w_glu", "zT2"], writes=["pq%d" % (4 + oc // 4)])
        for oc in range(NJ):
            p.op("act", lambda e, oc=oc: e.activation(out=sig[:, oc, :], in_=pq[4 + oc // 4][:, (oc % 4) * 128:(oc % 4 + 1) * 128], func=AF.Sigmoid, bias=b_glu[:, oc:oc + 1]),
                 reads=["pq%d" % (4 + oc // 4), "b_glu"], writes=["sig"])
        tt(so[:], zT2[:], sig[:], ALU.mult, ["zT2", "sig"], ["so"])
        for oc in range(8):
            pt = pq[oc // 4]
            for kc in range(NJ):
                p.op("pe", lambda e, oc=oc, kc=kc, pt=pt: e.matmul(pt[:, (oc % 4) * 128:(oc % 4 + 1) * 128], lhsT=w_brs[:, kc, oc * 128:(oc + 1) * 128], rhs=so[:, kc, :],
                                                                   start=(kc == 0), stop=(kc == NJ - 1)), reads=["w_brs", "so"], writes=["pq%d" % (oc // 4)])
            pt2 = pq[2 + oc // 4]
            for kc in range(8):
                p.op("pe", lambda e, oc=oc, kc=kc, pt2=pt2: e.matmul(pt2[:, (oc % 4) * 128:(oc % 4 + 1) * 128], lhsT=w_bra[:, kc, oc * 128:(oc + 1) * 128], rhs=aT2[:, kc, :],
                                                                     start=(kc == 0), stop=(kc == 7)), reads=["w_bra", "aT2"], writes=["pq%d" % (2 + oc // 4)])
        for b2 in range(2):
            tt(mixa[:, b2 * 4:b2 * 4 + 4, :].rearrange("p a t -> p (a t)"), pq[b2][:], sg[:, b2 * 4:b2 * 4 + 4, :].rearrange("p a t -> p (a t)"), ALU.mult,
               ["pq%d" % b2, "sg"], ["mixa"])
            tt(mixb[:, b2 * 4:b2 * 4 + 4, :].rearrange("p a t -> p (a t)"), pq[2 + b2][:], sg[:, 8 + b2 * 4:8 + b2 * 4 + 4, :].rearrange("p a t -> p (a t)"), ALU.mult,
               ["pq%d" % (2 + b2), "sg"], ["mixb"])
        tt(mixT[:], mixa[:], mixb[:], ALU.add, ["mixa", "mixb"], ["mixT"])
        for hf in range(2):
            for kc in range(8):
                p.op("pe", lambda e, hf=hf, kc=kc: e.matmul(pq[4 + hf][:], lhsT=mixT[:, kc, :], rhs=w_out[:, kc, hf * 512:(hf + 1) * 512], start=(kc == 0), stop=(kc == 7)),
                     reads=["mixT", "w_out"], writes=["pq%d" % (4 + hf)])
            tt(h2[:, hf * 512:(hf + 1) * 512], pq[4 + hf][:], x2[:, hf * 512:(hf + 1) * 512], ALU.add, ["pq%d" % (4 + hf), "x2"], ["h2"])
        p.dma("sp", h2_d[ob * 128:(ob + 1) * 128, :], h2[:], reads=["h2"], writes=["h2_d"])
        if debug:
            p.dma("sp", dbg["h2"][ob * 128:(ob + 1) * 128, :], h2[:], reads=["h2"], writes=["dbg_h2"])
        p.op("act", lambda e: e.activation(out=junk2[:], in_=h2[:], func=AF.Square, accum_out=ss2[:]), reads=["h2"], writes=["junk2", "ss2"])
        p.op("act", lambda e: e.activation(out=rstd2[:], in_=ss2[:], func=AF.Sqrt, scale=1.0 / 1024, bias=EPS), reads=["ss2"], writes=["rstd2"])
        dv(lambda e: e.reciprocal(out=rstd2[:], in_=rstd2[:]), ["rstd2"], ["rstd2"])
        dv(lambda e: e.scalar_tensor_tensor(out=xn[:], in0=h2[:], scalar=rstd2[:, 0:1], in1=g_ffn[:], op0=ALU.mult, op1=ALU.mult), ["h2", "rstd2", "g_ffn"], ["xn"])
        dv(lambda e: e.tensor_copy(out=xnb[:], in_=xn[:]), ["xn"], ["xnb"], eng="pool")
        for kc in range(8):
            p.op("pe", lambda e, kc=kc: e.transpose(out=pq[6 + kc // 4][:, (kc % 4) * 128:(kc % 4 + 1) * 128], in_=xn[:, kc * 128:(kc + 1) * 128], identity=identf[:]),
                 reads=["xn", "identf"], writes=["pq%d" % (6 + kc // 4)])
        for b2 in range(2):
            p.op("act", lambda e, b2=b2: e.activation(out=xnT[:, b2 * 4:b2 * 4 + 4, :].rearrange("p a t -> p (a t)"), in_=pq[6 + b2][:], func=AF.Copy),
                 reads=["pq%d" % (6 + b2)], writes=["xnT"])
        for kc in range(8):
            p.op("pe", lambda e, kc=kc: e.matmul(pq[6][:, 0:32], lhsT=xnT[:, kc, :], rhs=w_rt[:, kc, :], start=(kc == 0), stop=(kc == 7)),
                 reads=["xnT", "w_rt"], writes=["pq6"])
        tt(lg[:], pq[6][:, 0:32], b_rt[:], ALU.add, ["pq6", "b_rt"], ["lg"])
        dv(lambda e: e.max(out=top8[:], in_=lg[:]), ["lg"], ["top8"])
        dv(lambda e: e.max_index(out=idx8[:], in_max=top8[:], in_values=lg[:]), ["lg", "top8"], ["idx8"])
        dv(lambda e, ob=ob: e.tensor_copy(out=idx_all[:, ob, :], in_=idx8[:, 0:4]), ["idx8"], ["idx_all"])
        dv(lambda e: e.tensor_scalar(out=nmx[:], in0=top8[:, 0:1], scalar1=-1.0, scalar2=None, op0=ALU.mult), ["top8"], ["nmx"])
        p.op("act", lambda e: e.activation(out=ex4[:], in_=top8[:, 0:4], func=AF.Exp, bias=nmx[:, 0:1], accum_out=sm4[:]), reads=["top8", "nmx"], writes=["ex4", "sm4"])
        dv(lambda e: e.reciprocal(out=sm4[:], in_=sm4[:]), ["sm4"], ["sm4"])
        dv(lambda e, ob=ob: e.tensor_scalar(out=gate_all[:, ob, :], in0=ex4[:], scalar1=sm4[:, 0:1], scalar2=None, op0=ALU.mult), ["ex4", "sm4"], ["gate_all"])
        dv(lambda e: e.tensor_scalar(out=Mb[:], in0=lg[:], scalar1=top8[:, 3:4], scalar2=None, op0=ALU.is_ge), ["lg", "top8"], ["Mb"])
        p.op("pe", lambda e: e.matmul(pq[7][:, 0:32], lhsT=tri[:], rhs=Mb[:], start=True, stop=True), reads=["tri", "Mb"], writes=["pq7"])
        p.op("pe", lambda e: e.matmul(pq[7][:, 32:64], lhsT=ones[:], rhs=Mb[:], start=True, stop=True), reads=["ones", "Mb"], writes=["pq7"])
        tt(pos[:], pq[7][:, 0:32], base[:], ALU.add, ["pq7", "base"], ["pos"])
        tt(base[:], pq[7][:, 32:64], base[:], ALU.add, ["pq7", "base", "pos"], ["base"])
        for k4 in range(4):
            dv(lambda e, k4=k4, ob=ob: e.tensor_scalar(out=oh[:], in0=iota_f[:], scalar1=idx_all[:, ob, k4:k4 + 1], scalar2=None, op0=ALU.is_equal),
               ["iota_f", "idx_all"], ["oh"])
            tt(oh[:], oh[:], pos[:], ALU.mult, ["oh", "pos"], ["oh"])
            dv(lambda e, k4=k4: e.reduce_sum(out=sp4[:, k4:k4 + 1], in_=oh[:], axis=AX.X), ["oh"], ["sp4"])
        dv(lambda e, ob=ob: e.scalar_tensor_tensor(out=slf[:], in0=idx_all[:, ob, :], scalar=float(CAP), in1=sp4[:], op0=ALU.mult, op1=ALU.add),
           ["idx_all", "sp4"], ["slf"])
        dv(lambda e, ob=ob: e.tensor_copy(out=slot_all[:, ob, :], in_=slf[:]), ["slf"], ["slot_all"])
        for k4 in range(4):
            p.dma("pool", None, None, reads=["slot_all", "xnb"], writes=["Xs_d"], stream="scat",
                  fn=lambda e, ob=ob, k4=k4: e.indirect_dma_start(out=Xs_d, out_offset=bass.IndirectOffsetOnAxis(ap=slot_all[:, ob, k4:k4 + 1], axis=0),
                                                                   in_=xnb[:], in_offset=None))
    p.fence()
    p.flush()
    if stop == "p2":
        return nc
    P2.close()

    P3 = ExitStack()
    b_gu = load(P3, "b_gu", [128, 32, 16], F32, D["b_gu"])
    wgu = [p.sb(P3, "wgu%d" % i, [128, 8, 2048], BF16) for i in range(2)]
    wdn = [p.sb(P3, "wdn%d" % i, [128, 8, 1024], BF16) for i in range(2)]
    xe = [p.sb(P3, "xe%d" % i, [128, 1024], BF16) for i in range(3)]
    xeT = p.sb(P3, "xeT", [128, 8, CAP], BF16)
    hid = p.sb(P3, "hid", [128, 8, CAP], BF16)
    gc = p.sb(P3, "gc", [128, CAP], F32)
    sgm = p.sb(P3, "sgm", [128, CAP], F32)
    uc = p.sb(P3, "uc", [128, CAP], F32)
    ysl = [p.sb(P3, "ysl%d" % i, [128, 1024], F32) for i in range(2)]
    pg = [p.ps(P3, "pg%d" % i, [128, 512], F32) for i in range(4)]
    pd = [p.ps(P3, "pd%d" % i, [128, 512], F32) for i in range(2)]
    pt3 = p.ps(P3, "pt3", [128, 1024], BF16)
    NST = CAP // 128
    def load_w(ex):
        wi = ex % 2
        for kc in range(8):
            p.dma("pool", wgu[wi][:, kc, :], D["w_gu"][ex, kc * 128:(kc + 1) * 128, :], writes=["wgu%d" % wi], stream="wgu%d" % wi)
        p.dma("sp", wdf[:], D["w_dn"][ex].rearrange("(k p) n -> p k n", p=128), writes=["wdf"], stream="wdf")
    wdf = p.sb(P3, "wdf", [128, 8, 1024], F32)

    def cast_w(ex):
        wi = ex % 2
        p.op("act", lambda e, wi=wi: e.activation(out=wdn[wi][:].rearrange("p k n -> p (k n)"), in_=wdf[:].rearrange("p k n -> p (k n)"), func=AF.Copy),
             reads=["wdf"], writes=["wdn%d" % wi])
    xeT2 = [xeT, p.sb(P3, "xeTb", [128, 8, CAP], BF16)]

    def load_x(ex):
        xt = xeT2[ex % 2]
        for st_ in range(NST):
            p.dma("sp", xe[st_][:], Xs_d[ex * CAP + st_ * 128: ex * CAP + (st_ + 1) * 128, :], reads=["Xs_d"], writes=["xe%d" % st_])
            for kc in range(8):
                p.op("pe", lambda e, st_=st_, kc=kc: e.transpose(out=pt3[:, kc * 128:(kc + 1) * 128], in_=xe[st_][:, kc * 128:(kc + 1) * 128], identity=ident[:]),
                     reads=["xe%d" % st_, "ident"], writes=["pt3"])
            dv(lambda e, st_=st_, xt=xt: e.tensor_copy(out=xt[:, :, st_ * 128:(st_ + 1) * 128], in_=pt3[:].rearrange("p (k t) -> p k t", k=8)), ["pt3"], ["xeT%d" % (ex % 2)])
    load_w(0)
    cast_w(0)
    load_x(0)
    for ex in range(NE):
        wi = ex % 2
        xeT = xeT2[ex % 2]
        kxe = "xeT%d" % (ex % 2)
        if ex + 1 < NE:
            load_w(ex + 1)
        for fc in range(8):
            pgt = pg[(fc % 2) * 2]
            put = pg[(fc % 2) * 2 + 1]
            for kc in range(8):
                p.op("pe", lambda e, fc=fc, kc=kc, pgt=pgt, wi=wi, xeT=xeT: e.matmul(pgt[:, 0:CAP], lhsT=wgu[wi][:, kc, fc * 128:(fc + 1) * 128], rhs=xeT[:, kc, :], start=(kc == 0), stop=(kc == 7)),
                     reads=["wgu%d" % wi, kxe], writes=["pg%d" % ((fc % 2) * 2)])
            for kc in range(8):
                p.op("pe", lambda e, fc=fc, kc=kc, put=put, wi=wi, xeT=xeT: e.matmul(put[:, 0:CAP], lhsT=wgu[wi][:, kc, 1024 + fc * 128:1024 + (fc + 1) * 128], rhs=xeT[:, kc, :], start=(kc == 0), stop=(kc == 7)),
                     reads=["wgu%d" % wi, kxe], writes=["pg%d" % ((fc % 2) * 2 + 1)])
            dv(lambda e, ex=ex, fc=fc, pgt=pgt: e.tensor_scalar(out=gc[:], in0=pgt[:, 0:CAP], scalar1=b_gu[:, ex, fc:fc + 1], scalar2=7.0, op0=ALU.add, op1=ALU.min),
               ["pg%d" % ((fc % 2) * 2), "b_gu"], ["gc"])
            p.op("act", lambda e: e.activation(out=sgm[:], in_=gc[:], func=AF.Sigmoid, scale=1.702), reads=["gc"], writes=["sgm"])
            dv(lambda e, ex=ex, fc=fc, put=put: e.tensor_scalar(out=uc[:], in0=put[:, 0:CAP], scalar1=b_gu[:, ex, 8 + fc:9 + fc], scalar2=7.0, op0=ALU.add, op1=ALU.min),
               ["pg%d" % ((fc % 2) * 2 + 1), "b_gu"], ["uc"])
            dv(lambda e: e.tensor_scalar(out=uc[:], in0=uc[:], scalar1=-7.0, scalar2=1.0, op0=ALU.max, op1=ALU.add), ["uc"], ["uc"])
            tt(gc[:], gc[:], sgm[:], ALU.mult, ["gc", "sgm"], ["gc"])
            tt(hid[:, fc, :], gc[:], uc[:], ALU.mult, ["gc", "uc"], ["hid"])
        if ex + 1 < NE:
            load_x(ex + 1)
        for st_ in range(NST):
            yt = ysl[st_ % 2]
            for hf in range(2):
                for fc in range(8):
                    p.op("pe", lambda e, st_=st_, hf=hf, fc=fc, wi=wi: e.matmul(pd[hf][:], lhsT=hid[:, fc, st_ * 128:(st_ + 1) * 128], rhs=wdn[wi][:, fc, hf * 512:(hf + 1) * 512],
                                                                         start=(fc == 0), stop=(fc == 7)), reads=["hid", "wdn%d" % wi], writes=["pd%d" % hf])
                p.op("act", lambda e, hf=hf, yt=yt: e.activation(out=yt[:, hf * 512:(hf + 1) * 512], in_=pd[hf][:], func=AF.Copy), reads=["pd%d" % hf], writes=["ysl%d" % (st_ % 2)])
            p.dma("sp", Ys_d[ex * CAP + st_ * 128: ex * CAP + (st_ + 1) * 128, :], yt[:], reads=["ysl%d" % (st_ % 2)], writes=["Ys_d"])
        if ex + 1 < NE:
            cast_w(ex + 1)
    p.fence()
    p.flush()
    if stop == "p3":
        return nc
    P3.close()

    P4 = ExitStack()
    b_dn = load(P4, "b_dn", [32, 1024], F32, D["b_dn"])
    g_fin = load(P4, "g_fin", [128, 1024], F32, D["g_fin"])
    iota_i4 = p.sb(P4, "iota_i4", [128, 32], I32)
    iota_f4 = p.sb(P4, "iota_f4", [128, 32], F32)
    p.op("pool", lambda e: e.iota(iota_i4[:], pattern=[[1, 32]], base=0, channel_multiplier=0), writes=["iota_i4"])
    dv(lambda e: e.tensor_copy(out=iota_f4[:], in_=iota_i4[:]), ["iota_i4"], ["iota_f4"])
    rows = [p.sb(P4, "rows%d" % i, [128, 1024], F32) for i in range(4)]
    h2r = p.sb(P4, "h2r", [128, 1024], F32)
    acc = p.sb(P4, "acc", [128, 1024], F32)
    Gm = p.sb(P4, "Gm", [128, 32], F32)
    oh4 = p.sb(P4, "oh4", [128, 32], F32)
    GmT = p.sb(P4, "GmT", [32, 128], F32)
    junk4 = p.sb(P4, "junk4", [128, 1024], F32)
    ss4 = p.sb(P4, "ss4", [128, 1], F32)
    rstd4 = p.sb(P4, "rstd4", [128, 1], F32)
    ot = p.sb(P4, "ot", [128, 1024], F32)
    pb4 = [p.ps(P4, "pb4%d" % i, [128, 512], F32) for i in range(2)]
    pgt4 = p.ps(P4, "pgt4", [128, 512], F32)
    for ob in range(NB_OWN):
        p.dma("sp", h2r[:], h2_d[ob * 128:(ob + 1) * 128, :], reads=["h2_d"], writes=["h2r"])
        for k4 in range(4):
            p.dma("pool", None, None, reads=["slot_all", "Ys_d"], writes=["rows%d" % k4], stream="gath%d" % k4,
                  fn=lambda e, ob=ob, k4=k4: e.indirect_dma_start(out=rows[k4][:], out_offset=None, in_=Ys_d,
                                                                   in_offset=bass.IndirectOffsetOnAxis(ap=slot_all[:, ob, k4:k4 + 1], axis=0)))
        dv(lambda e: e.memset(Gm[:], 0.0), [], ["Gm"])
        for k4 in range(4):
            dv(lambda e, ob=ob, k4=k4: e.tensor_scalar(out=oh4[:], in0=iota_f4[:], scalar1=idx_all[:, ob, k4:k4 + 1], scalar2=gate_all[:, ob, k4:k4 + 1],
                                                       op0=ALU.is_equal, op1=ALU.mult), ["iota_f4", "idx_all", "gate_all"], ["oh4"])
            tt(Gm[:], Gm[:], oh4[:], ALU.add, ["Gm", "oh4"], ["Gm"])
        p.op("pe", lambda e: e.transpose(out=pgt4[0:32, 0:128], in_=Gm[:], identity=identf[:]), reads=["Gm", "identf"], writes=["pgt4"])
        p.op("act", lambda e: e.activation(out=GmT[:], in_=pgt4[0:32, 0:128], func=AF.Copy), reads=["pgt4"], writes=["GmT"])
        for hf in range(2):
            p.op("pe", lambda e, hf=hf: e.matmul(pb4[hf][:], lhsT=GmT[:], rhs=b_dn[:, hf * 512:(hf + 1) * 512], start=True, stop=True),
                 reads=["GmT", "b_dn"], writes=["pb4%d" % hf])
            tt(acc[:, hf * 512:(hf + 1) * 512], pb4[hf][:], h2r[:, hf * 512:(hf + 1) * 512], ALU.add, ["pb4%d" % hf, "h2r"], ["acc"])
        for k4 in range(4):
            dv(lambda e, ob=ob, k4=k4: e.scalar_tensor_tensor(out=acc[:], in0=rows[k4][:], scalar=gate_all[:, ob, k4:k4 + 1], in1=acc[:], op0=ALU.mult, op1=ALU.add),
               ["rows%d" % k4, "gate_all", "acc"], ["acc"])
        p.op("act", lambda e: e.activation(out=junk4[:], in_=acc[:], func=AF.Square, accum_out=ss4[:]), reads=["acc"], writes=["junk4", "ss4"])
        p.op("act", lambda e: e.activation(out=rstd4[:], in_=ss4[:], func=AF.Sqrt, scale=1.0 / 1024, bias=EPS), reads=["ss4"], writes=["rstd4"])
        dv(lambda e: e.reciprocal(out=rstd4[:], in_=rstd4[:]), ["rstd4"], ["rstd4"])
        dv(lambda e: e.scalar_tensor_tensor(out=ot[:], in0=acc[:], scalar=rstd4[:, 0:1], in1=g_fin[:], op0=ALU.mult, op1=ALU.mult), ["acc", "rstd4", "g_fin"], ["ot"])
        p.dma("sp", out[ob * 128:(ob + 1) * 128, :], ot[:], reads=["ot"], writes=["out"])
    p.fence()
    p.flush()
    P4.close()
    G.close()
    p.stack.close()
    return nc


_CACHE = {}


def kernel(**inputs):
    per = _host_prep(inputs)
    shapes = {k: v.shape for k, v in per[0].items()}
    nc = build_nc(shapes)
    res = run_bass_kernel_spmd(nc, per, core_ids=list(range(8)))
    outs = [np.asarray(r["out"], np.float32) for r in res.results]
    o = np.stack(outs, 0).reshape(2, 4 * NB_OWN * 128, 1024)
    return o
```

```python
import contextlib
from contextlib import ExitStack
import numpy as np
import concourse.bass as bass
import concourse.mybir as mybir

F32 = mybir.dt.float32
BF16 = mybir.dt.bfloat16
I32 = mybir.dt.int32
U32 = mybir.dt.uint32
AF = mybir.ActivationFunctionType
ALU = mybir.AluOpType
AX = mybir.AxisListType

SELF_SYNC = True


class Prog:
    ENG = ("pe", "act", "dve", "pool", "sp")

    def __init__(self, nc):
        self.nc = nc
        self.eobj = {"pe": nc.tensor, "act": nc.scalar, "dve": nc.vector,
                     "pool": nc.gpsimd, "sp": nc.sync}
        self.stack = contextlib.ExitStack()
        self.sems = {}
        self.cnt = {}
        self.ops = {e: [] for e in self.ENG}
        self.seen = {e: {} for e in self.ENG}
        self.lastw = {}
        self.readers = {}
        self.nops = 0

    def sem(self, name):
        if name not in self.sems:
            self.sems[name] = self.stack.enter_context(self.nc.semaphore(name))
            self.cnt[name] = 0
        return self.sems[name]

    def sb(self, st, name, shape, dt):
        return st.enter_context(self.nc.sbuf_tensor("s_" + name, list(shape), dt))

    def ps(self, st, name, shape, dt=F32):
        return st.enter_context(self.nc.psum_tensor("p_" + name, list(shape), dt))

    def _deps(self, eng, reads, writes):
        deps = {}
        def add(tok):
            if tok is None:
                return
            s, v = tok
            if s.startswith("d_"):
                v = self.cnt[s]
            if s == "c_" + eng and (eng == "pe" or not SELF_SYNC):
                return
            if deps.get(s, 0) < v:
                deps[s] = v
        for k in reads:
            add(self.lastw.get(k))
        for k in writes:
            add(self.lastw.get(k))
            for t in self.readers.get(k, ()):
                add(t)
        out = []
        seen = self.seen[eng]
        for s, v in deps.items():
            if seen.get(s, 0) < v:
                seen[s] = v
                out.append((s, v))
        return out

    def _commit(self, tok, reads, writes):
        for k in writes:
            self.lastw[k] = tok
            self.readers[k] = []
        for k in reads:
            self.readers.setdefault(k, []).append(tok)
            if len(self.readers[k]) > 64:
                m = {}
                for s, v in self.readers[k]:
                    if m.get(s, 0) < v:
                        m[s] = v
                self.readers[k] = list(m.items())

    def op(self, eng, fn, reads=(), writes=()):
        s = "c_" + eng
        self.sem(s)
        waits = self._deps(eng, reads, writes)
        self.cnt[s] += 1
        tok = (s, self.cnt[s])
        self.ops[eng].append((waits, fn, s, 1))
        self._commit(tok, reads, writes)
        self.nops += 1
        return tok

    def dma(self, q, out, in_, reads=(), writes=(), stream=None, fn=None):
        s = "d_" + (stream or (str(writes[0]) if writes else "misc"))
        s = s.replace(" ", "").replace("'", "").replace(",", "_").replace("(", "").replace(")", "")
        self.sem(s)
        waits = self._deps(q, reads, writes)
        self.cnt[s] += 16
        tok = (s, self.cnt[s])
        if fn is None:
            fn = lambda e, out=out, in_=in_: e.dma_start(out=out, in_=in_)
        self.ops[q].append((waits, fn, s, 16))
        self._commit(tok, reads, writes)
        self.nops += 1
        return tok

    def fence(self):
        allt = [(s, v) for s, v in self.cnt.items() if v > 0]
        for e in self.ENG:
            waits = []
            for s, v in allt:
                if self.seen[e].get(s, 0) < v:
                    self.seen[e][s] = v
                    waits.append((s, v))
            if waits:
                self.ops[e].append((waits, None, None, 0))
        self.lastw.clear()
        self.readers.clear()

    def flush(self):
        ops = self.ops
        sems = self.sems
        eobj = self.eobj

        def replay(name):
            def f(e):
                for waits, fn, s, inc in ops[name]:
                    for (ws, wv) in waits:
                        e.wait_ge(sems[ws], wv)
                    if fn is not None:
                        ins = fn(e)
                        ins.then_inc(sems[s], inc)
            return f
        with self.nc.Block() as block:
            block.tensor(replay("pe"))
            block.scalar(replay("act"))
            block.vector(replay("dve"))
            block.gpsimd(replay("pool"))
            block.sync(replay("sp"))
        self.ops = {e: [] for e in self.ENG}

    def finish(self):
        self.fence()
        self.flush()
        self.stack.close()

from concourse.bass_utils import run_bass_kernel_spmd
import math

NB_CTX = 49
CTX_FAST = True
SUB = {"u", "ssm", "ssmown", "dma", "qkv", "attn"}
NB_OWN = 16
NB = NB_CTX + NB_OWN
CAP = 384
NE = 32
NJ = 11
EPS = 1e-5


def _host_prep(inp):
    f = np.float32
    x = np.asarray(inp["x"], f)
    meta = np.asarray(inp["meta_tokens"], f)
    w_in = np.asarray(inp["w_in"], f)[0]
    chmap = np.full(NJ * 128, -1, np.int64)
    for pc in range(NJ * 128):
        j, r = divmod(pc, 128)
        pi = 3 * j + r // 32
        if r < 96 and pi < 32:
            chmap[pc] = 32 * pi + r % 32
    val = chmap >= 0

    def padcols(w):
        o = np.zeros(w.shape[:-1] + (NJ * 128,), f)
        o[..., val] = w[..., chmap[val]]
        return o

    def padrows(w):
        o = np.zeros((NJ * 128,) + w.shape[1:], f)
        o[val] = w[chmap[val]]
        return o

    def kmaj(w):
        K, N = w.shape
        return np.ascontiguousarray(w.reshape(K // 128, 128, N).transpose(1, 0, 2))

    com = {}
    com["w_u"] = kmaj(padcols(w_in[:, 0:1024]))
    com["w_qkv"] = kmaj(w_in[:, 1024:2304])
    com["w_g"] = kmaj(w_in[:, 2304:4352])
    com["w_glu"] = kmaj(padrows(padcols(np.asarray(inp["w_glu"], f)[0])))
    com["w_brs"] = kmaj(padrows(np.asarray(inp["w_br_ssm"], f)[0]))
    com["w_bra"] = kmaj(np.asarray(inp["w_br_attn"], f)[0])
    com["w_out"] = kmaj(np.asarray(inp["w_out"], f)[0])
    com["w_rt"] = kmaj(np.asarray(inp["w_router"], f)[0])
    com["b_glu"] = np.ascontiguousarray(padcols(np.asarray(inp["b_glu"], f)[0][None])[0].reshape(NJ, 128).T)
    com["d_pad"] = np.ascontiguousarray(padcols(np.asarray(inp["ssm_d"], f)[0].reshape(1, 1024))[0].reshape(NJ, 128).T)
    com["g_mix"] = np.ascontiguousarray(np.asarray(inp["norm_mix"], f)[0].reshape(8, 128).T)
    com["g_ffn"] = np.ascontiguousarray(np.broadcast_to(np.asarray(inp["norm_ffn"], f)[0][None], (128, 1024)))
    com["g_fin"] = np.ascontiguousarray(np.broadcast_to(np.asarray(inp["norm_final"], f)[None], (128, 1024)))
    com["b_rt"] = np.ascontiguousarray(np.broadcast_to(np.asarray(inp["b_router"], f)[0][None], (128, 32)))
    com["sinks"] = np.ascontiguousarray(np.broadcast_to(np.asarray(inp["attn_sinks"], f)[0][None], (128, 16)))
    com["w_gu"] = np.asarray(inp["w_gate_up"], f)[0]
    com["w_dn"] = np.asarray(inp["w_down"], f)[0]
    com["b_gu"] = np.ascontiguousarray(np.asarray(inp["b_gate_up"], f)[0].reshape(32, 16, 128).transpose(2, 0, 1))
    com["b_dn"] = np.ascontiguousarray(np.asarray(inp["b_down"], f)[0])
    lre = np.asarray(inp["ssm_lam_re"], f)[0]; lim = np.asarray(inp["ssm_lam_im"], f)[0]
    ldt = np.asarray(inp["ssm_log_dt"], f)[0]
    bre = np.asarray(inp["ssm_b_re"], f)[0]; bim = np.asarray(inp["ssm_b_im"], f)[0]
    cre = np.asarray(inp["ssm_c_re"], f)[0]; cim = np.asarray(inp["ssm_c_im"], f)[0]
    sig = np.arange(128); sb_ = sig // 64; sp_ = sig % 64
    pi_ = np.arange(32)
    gS = 2 * pi_[None, :] + sb_[:, None]
    com["lre_S"] = np.ascontiguousarray(lre[gS, sp_[:, None]])
    com["lim_S"] = np.ascontiguousarray(lim[gS, sp_[:, None]])
    com["ldt_S"] = np.ascontiguousarray(ldt[gS])
    c32 = np.arange(32); cb = c32 // 16; chh = c32 % 16
    mC = (cb[None, None, :] == sb_[:, None, None])
    com["cre_S"] = np.ascontiguousarray(np.where(mC, cre[gS[:, :, None], chh[None, None, :], sp_[:, None, None]], 0).astype(f))
    com["cim_S"] = np.ascontiguousarray(np.where(mC, cim[gS[:, :, None], chh[None, None, :], sp_[:, None, None]], 0).astype(f))
    com["bre_S"] = np.ascontiguousarray(np.where(mC, bre[gS[:, :, None], sp_[:, None, None], chh[None, None, :]], 0).astype(f))
    com["bim_S"] = np.ascontiguousarray(np.where(mC, bim[gS[:, :, None], sp_[:, None, None], chh[None, None, :]], 0).astype(f))
    com["w_u0"] = kmaj(w_in[:, 0:1024])
    chp = np.arange(128); jj = np.arange(NJ)
    piR = 3 * jj[None, :] + (chp // 32)[:, None]
    vR = (chp[:, None] < 96) & (piR < 32)
    piRc = np.where(vR, piR, 0)
    gR = 2 * piRc[:, :, None] + sb_[None, None, :]
    vR3 = np.broadcast_to(vR[:, :, None], gR.shape)
    com["lre_R"] = np.ascontiguousarray(np.where(vR3, lre[gR, sp_[None, None, :]], -0.5).astype(f))
    com["lim_R"] = np.ascontiguousarray(np.where(vR3, lim[gR, sp_[None, None, :]], 1.0).astype(f))
    com["ldt_R"] = np.ascontiguousarray(np.where(vR3, ldt[gR], math.log(0.01)).astype(f))
    bch = ((chp % 32) // 16)[:, None, None]; hh = (chp % 16)[:, None, None]
    mB = vR3 & (bch == sb_[None, None, :])
    com["bre_R"] = np.ascontiguousarray(np.where(mB, bre[gR, sp_[None, None, :], hh], 0).astype(f))
    com["bim_R"] = np.ascontiguousarray(np.where(mB, bim[gR, sp_[None, None, :], hh], 0).astype(f))
    kq = np.arange(128)
    com["m_cur"] = (kq[:, None] <= kq[None, :]).astype(f)
    com["m_prev"] = (kq[:, None] > kq[None, :]).astype(f)
    com["tri"] = (kq[:, None] < kq[None, :]).astype(f)
    inv_freq = (500000.0 ** (-np.arange(0, 16, 2, dtype=f) / f(16))).astype(f)
    metablk = np.zeros((128, 1024), f); metablk[:16] = meta
    per = []
    for c in range(8):
        b, k = divmod(c, 4)
        seq = np.concatenate([meta, x[b]], 0)
        plen = 16 + 2048 * k
        npad = NB_CTX * 128 - plen
        xl = np.zeros((NB * 128, 1024), f)
        xl[npad:] = seq[:plen + NB_OWN * 128]
        pos = (np.arange(NB * 128) - npad).clip(0).astype(f)
        pos = np.concatenate([np.arange(128, dtype=f), pos])
        ang = pos[:, None] * inv_freq[None, :]
        d = dict(com)
        d["xloc"] = xl
        d["xmeta"] = metablk
        d["rcos"] = np.cos(ang).astype(f).reshape(NB + 1, 128, 8)
        d["rsin"] = np.sin(ang).astype(f).reshape(NB + 1, 128, 8)
        d["m_first"] = com["m_prev"] * f(1.0 if k > 0 else 0.0)
        per.append(d)
    return per


def build_nc(shapes, debug=False, stop=None, nblk=None):
    nc = bass.Bass("TRN2", target_bir_lowering=False)
    D = {}
    for name, shp in shapes.items():
        D[name] = nc.dram_tensor(name, list(shp), F32, kind="ExternalInput").ap()
    out = nc.dram_tensor("out", [NB_OWN * 128, 1024], F32, kind="ExternalOutput").ap()
    NT = NB_OWN * 128

    def scr(name, shape, dt):
        return nc.dram_tensor(name, list(shape), dt, kind="Internal").ap()
    zT_d = scr("zT_d", [NB_OWN, 128, NJ * 128], BF16)
    aT_d = scr("aT_d", [NB_OWN, 128, 1024], BF16)
    hT_d = scr("hT_d", [NB_OWN, 128, 1024], BF16)
    h2_d = scr("h2_d", [NT, 1024], F32)
    Xs_d = scr("Xs_d", [NE * CAP + 128, 1024], BF16)
    Ys_d = scr("Ys_d", [NE * CAP, 1024], F32)
    dbg = {}
    if debug:
        dbg["h2"] = nc.dram_tensor("dbg_h2", [NT, 1024], F32, kind="ExternalOutput").ap()

    p = Prog(nc)
    G = ExitStack()
    ident = p.sb(G, "ident", [128, 128], BF16)
    identf = p.sb(G, "identf", [128, 128], F32)
    p.op("pool", lambda e: e.memset(identf[:], 1.0), writes=["identf"])
    p.op("pool", lambda e: e.affine_select(out=identf[:], in_=identf[:], pattern=[[1, 128]], compare_op=ALU.is_equal,
                                           fill=0.0, base=0, channel_multiplier=-1), reads=["identf"], writes=["identf"])
    p.op("dve", lambda e: e.tensor_copy(out=ident[:], in_=identf[:]), reads=["identf"], writes=["ident"])
    slot_all = p.sb(G, "slot_all", [128, NB_OWN, 4], I32)
    gate_all = p.sb(G, "gate_all", [128, NB_OWN, 4], F32)
    idx_all = p.sb(G, "idx_all", [128, NB_OWN, 4], F32)

    def load(st, name, shape, dt, src, q="sp"):
        t = p.sb(st, name, shape, dt)
        p.dma(q, t[:], src, writes=[name])
        return t

    P1 = ExitStack()
    w_u = load(P1, "w_u", [128, 8, NJ * 128], BF16, D["w_u"], "pool")
    g_mix = load(P1, "g_mix", [128, 8], F32, D["g_mix"])
    d_pad = load(P1, "d_pad", [128, NJ], F32, D["d_pad"])
    m_cur = load(P1, "m_cur", [128, 128], BF16, D["m_cur"], "pool")
    m_prev = load(P1, "m_prev", [128, 128], BF16, D["m_prev"], "pool")
    m_first = load(P1, "m_first", [128, 128], BF16, D["m_first"], "pool")
    esink = load(P1, "esink", [128, 16], F32, D["sinks"])
    p.op("act", lambda e: e.activation(out=esink[:], in_=esink[:], func=AF.Exp), reads=["esink"], writes=["esink"])

    zrow = p.sb(P1, "zrow", [128, 1024], BF16)
    p.op("pool", lambda e: e.memset(zrow[:], 0.0), writes=["zrow"])
    w_qkv = load(P1, "w_qkv", [128, 8, 1280], BF16, D["w_qkv"], "pool")
    BbP = [p.sb(P1, "BbPr", [128, NJ, 3, 128], BF16), p.sb(P1, "BbPi", [128, NJ, 3, 128], BF16)]
    CTP = [p.sb(P1, "CTPr", [128, 32, 128], BF16), p.sb(P1, "CTPi", [128, 32, 128], BF16)]
    for x_ in range(2):
        p.op("pool", lambda e, x_=x_: e.memset(BbP[x_][:], 0.0), writes=["BbP"])
        p.op("pool", lambda e, x_=x_: e.memset(CTP[x_][:], 0.0), writes=["CTP"])
    Xc = [p.sb(P1, "Xcr", [128, 32], F32), p.sb(P1, "Xci", [128, 32], F32)]
    aS = [p.sb(P1, "aSr", [128, 32], F32), p.sb(P1, "aSi", [128, 32], F32)]
    fS = [p.sb(P1, "fSr", [128, 32], F32), p.sb(P1, "fSi", [128, 32], F32)]
    p.op("dve", lambda e: e.memset(Xc[0][:], 0.0), writes=["Xc"])
    p.op("dve", lambda e: e.memset(Xc[1][:], 0.0), writes=["Xc"])

    def dv(fn, r, w, eng="dve"):
        return p.op(eng, fn, reads=r, writes=w)

    def tt(o, a, b, op, r, w, eng="dve"):
        return p.op(eng, lambda e: e.tensor_tensor(out=o, in0=a, in1=b, op=op), reads=r, writes=w)

    def cexp_tables(T, lre_ap, lim_ap, ldt_ap, shape, tag):
        t = {}
        for nm in ("lr", "li", "dt", "th", "s", "c", "t1", "t2", "t3", "mag", "ar", "ai", "fr", "fi", "den"):
            t[nm] = p.sb(T, tag + nm, shape, F32)
        k = [tag]
        p.dma("sp", t["lr"][:], lre_ap, writes=k)
        p.dma("sp", t["li"][:], lim_ap, writes=k)
        p.dma("sp", t["dt"][:], ldt_ap, writes=k)
        p.op("act", lambda e: e.activation(out=t["dt"][:], in_=t["dt"][:], func=AF.Exp), reads=k, writes=k)
        tt(t["th"][:], t["dt"][:], t["li"][:], ALU.mult, k, k)
        tt(t["t1"][:], t["dt"][:], t["lr"][:], ALU.mult, k, k)
        p.op("act", lambda e: e.activation(out=t["mag"][:], in_=t["t1"][:], func=AF.Exp), reads=k, writes=k)
        p.op("act", lambda e: e.activation(out=t["s"][:], in_=t["th"][:], func=AF.Sin, scale=1.0 / 16), reads=k, writes=k)
        p.op("act", lambda e: e.activation(out=t["t1"][:], in_=t["th"][:], func=AF.Sin, scale=1.0 / 32), reads=k, writes=k)
        tt(t["t1"][:], t["t1"][:], t["t1"][:], ALU.mult, k, k)
        dv(lambda e: e.tensor_scalar(out=t["c"][:], in0=t["t1"][:], scalar1=-2.0, scalar2=1.0, op0=ALU.mult, op1=ALU.add), k, k)
        for _ in range(4):
            tt(t["t1"][:], t["c"][:], t["c"][:], ALU.mult, k, k)
            tt(t["t2"][:], t["s"][:], t["s"][:], ALU.mult, k, k)
            tt(t["t3"][:], t["c"][:], t["s"][:], ALU.mult, k, k)
            tt(t["c"][:], t["t1"][:], t["t2"][:], ALU.subtract, k, k)
            dv(lambda e: e.tensor_scalar(out=t["s"][:], in0=t["t3"][:], scalar1=2.0, scalar2=None, op0=ALU.mult), k, k)
        tt(t["ar"][:], t["mag"][:], t["c"][:], ALU.mult, k, k)
        tt(t["ai"][:], t["mag"][:], t["s"][:], ALU.mult, k, k)
        tt(t["t1"][:], t["lr"][:], t["lr"][:], ALU.mult, k, k)
        tt(t["t2"][:], t["li"][:], t["li"][:], ALU.mult, k, k)
        tt(t["den"][:], t["t1"][:], t["t2"][:], ALU.add, k, k)
        dv(lambda e: e.reciprocal(out=t["den"][:], in_=t["den"][:]), k, k)
        dv(lambda e: e.tensor_scalar(out=t["t3"][:], in0=t["ar"][:], scalar1=-1.0, scalar2=None, op0=ALU.add), k, k)
        tt(t["t1"][:], t["t3"][:], t["lr"][:], ALU.mult, k, k)
        tt(t["t2"][:], t["ai"][:], t["li"][:], ALU.mult, k, k)
        tt(t["t1"][:], t["t1"][:], t["t2"][:], ALU.add, k, k)
        tt(t["fr"][:], t["t1"][:], t["den"][:], ALU.mult, k, k)
        tt(t["t1"][:], t["ai"][:], t["lr"][:], ALU.mult, k, k)
        tt(t["t2"][:], t["t3"][:], t["li"][:], ALU.mult, k, k)
        tt(t["t1"][:], t["t1"][:], t["t2"][:], ALU.subtract, k, k)
        tt(t["fi"][:], t["t1"][:], t["den"][:], ALU.mult, k, k)
        return t

    with ExitStack() as T:
        BbT = [p.sb(T, "BbTr", [128, NJ, 128], BF16), p.sb(T, "BbTi", [128, NJ, 128], BF16)]
        tr_ = cexp_tables(T, D["lre_R"].rearrange("p j s -> p (j s)"), D["lim_R"].rearrange("p j s -> p (j s)"),
                          D["ldt_R"].rearrange("p j s -> p (j s)"), [128, NJ * 128], "R")
        br = load(T, "brR", [128, NJ * 128], F32, D["bre_R"].rearrange("p j s -> p (j s)"))
        bi = load(T, "biR", [128, NJ * 128], F32, D["bim_R"].rearrange("p j s -> p (j s)"))
        k = ["R", "brR", "biR"]
        tt(tr_["t1"][:], tr_["fr"][:], br[:], ALU.mult, k, k)
        tt(tr_["t2"][:], tr_["fi"][:], bi[:], ALU.mult, k, k)
        tt(BbT[0][:].rearrange("p j s -> p (j s)"), tr_["t1"][:], tr_["t2"][:], ALU.subtract, k, ["BbT"])
        tt(tr_["t1"][:], tr_["fr"][:], bi[:], ALU.mult, k, k)
        tt(tr_["t2"][:], tr_["fi"][:], br[:], ALU.mult, k, k)
        tt(BbT[1][:].rearrange("p j s -> p (j s)"), tr_["t1"][:], tr_["t2"][:], ALU.add, k, ["BbT"])
        for x_ in range(2):
            for pl in range(3):
                dv(lambda e, x_=x_, pl=pl: e.tensor_copy(out=BbP[x_][32 * pl:32 * pl + 32, :, pl, :], in_=BbT[x_][32 * pl:32 * pl + 32, :, :]), ["BbT", "BbP"], ["BbP"])
        p.fence()
        p.flush()
    Ec = p.sb(P1, "Ec", [128, 32, 128], F32)
    Es = p.sb(P1, "Es", [128, 32, 128], F32)
    Rt = p.sb(P1, "Rt", [128, 32], F32)
    with ExitStack() as T:
        CT = [p.sb(T, "CTr", [128, 32, 32], BF16), p.sb(T, "CTi", [128, 32, 32], BF16)]
        ts_ = cexp_tables(T, D["lre_S"], D["lim_S"], D["ldt_S"], [128, 32], "S")
        k = ["S"]
        dv(lambda e: e.tensor_copy(out=Ec[:, :, 0], in_=ts_["c"][:]), k, ["E"])
        dv(lambda e: e.tensor_copy(out=Es[:, :, 0], in_=ts_["s"][:]), k, ["E"])
        tmpA = p.sb(T, "tmpA", [128, 32, 64], F32)
        tmpB = p.sb(T, "tmpB", [128, 32, 64], F32)
        m = 1
        while m < 128:
            cm = Ec[:, :, m - 1:m].to_broadcast([128, 32, m])
            sm = Es[:, :, m - 1:m].to_broadcast([128, 32, m])
            kk = ["E", "tmp"]
            tt(tmpA[:, :, 0:m], Ec[:, :, 0:m], cm, ALU.mult, kk, kk)
            tt(tmpB[:, :, 0:m], Es[:, :, 0:m], sm, ALU.mult, kk, kk)
            tt(Ec[:, :, m:2 * m], tmpA[:, :, 0:m], tmpB[:, :, 0:m], ALU.subtract, kk, kk)
            tt(tmpA[:, :, 0:m], Ec[:, :, 0:m], sm, ALU.mult, kk, kk)
            tt(tmpB[:, :, 0:m], Es[:, :, 0:m], cm, ALU.mult, kk, kk)
            tt(Es[:, :, m:2 * m], tmpA[:, :, 0:m], tmpB[:, :, 0:m], ALU.add, kk, kk)
            m *= 2
        dv(lambda e: e.tensor_copy(out=Rt[:], in_=ts_["mag"][:]), k, ["Rt"])
        dv(lambda e: e.tensor_copy(out=aS[0][:], in_=ts_["ar"][:]), k, ["aS"])
        dv(lambda e: e.tensor_copy(out=aS[1][:], in_=ts_["ai"][:]), k, ["aS"])
        dv(lambda e: e.tensor_copy(out=fS[0][:], in_=ts_["fr"][:]), k, ["fS"])
        dv(lambda e: e.tensor_copy(out=fS[1][:], in_=ts_["fi"][:]), k, ["fS"])
        cr = load(T, "crS", [128, 32, 32], F32, D["cre_S"])
        ci = load(T, "ciS", [128, 32, 32], F32, D["cim_S"])
        dv(lambda e: e.tensor_copy(out=CT[0][:], in_=cr[:]), ["crS"], ["CT"])
        dv(lambda e: e.tensor_scalar(out=CT[1][:], in0=ci[:], scalar1=-1.0, scalar2=None, op0=ALU.mult), ["ciS"], ["CT"])
        for x_ in range(2):
            for pl in range(3):
                npi = len(range(pl, 32, 3))
                dv(lambda e, x_=x_, pl=pl: e.tensor_copy(out=CTP[x_][:, pl::3, 32 * pl:32 * pl + 32], in_=CT[x_][:, pl::3, :]), ["CT", "CTP"], ["CTP"])
        p.fence()
        p.flush()


    if CTX_FAST:
      with ExitStack() as Q:
        GT = [p.sb(Q, "GTr", [128, 32, 128], BF16), p.sb(Q, "GTi", [128, 32, 128], BF16)]
        am = [p.sb(Q, "amr", [128, 32], F32), p.sb(Q, "ami", [128, 32], F32)]
        BbS = [p.sb(Q, "BbSr", [128, 32, 32], F32), p.sb(Q, "BbSi", [128, 32, 32], F32)]
        w_u0 = load(Q, "w_u0", [128, 8, 1024], BF16, D["w_u0"], "pool")
        qs_tr = p.ps(Q, "qs_tr", [128, 1024], BF16)
        qs_u = p.ps(Q, "qs_u", [128, 2, 512], F32)
        qs_m = [p.ps(Q, "qs_m%d" % i, [128, 2, 512], F32) for i in range(2)]
        Q2 = ExitStack()
        Gr = p.sb(Q2, "Gr", [128, 32, 128], F32)
        Gi = p.sb(Q2, "Gi", [128, 32, 128], F32)
        tq = [p.sb(Q2, "tq%d" % i, [128, 32, 64], F32) for i in range(2)]
        ts3 = [p.sb(Q2, "ts3%d" % i, [128, 32], F32) for i in range(3)]
        bSr = tq[0][:, :, 0:32]
        bSi = tq[0][:, :, 32:64]
        p.dma("sp", bSr, D["bre_S"], writes=["bSr"])
        p.dma("sp", bSi, D["bim_S"], writes=["bSi"])
        t_a = tq[1][:, :, 0:32]
        t_b = tq[1][:, :, 32:64]
        frb = fS[0][:].unsqueeze(2).to_broadcast([128, 32, 32])
        fib = fS[1][:].unsqueeze(2).to_broadcast([128, 32, 32])
        kb_ = ["fS", "bSr", "bSi", "tqb"]
        tt(t_a, bSr, frb, ALU.mult, kb_, ["tqb"])
        tt(t_b, bSi, fib, ALU.mult, kb_, ["tqb"])
        tt(BbS[0][:], t_a, t_b, ALU.subtract, ["tqb"], ["BbS"])
        tt(t_a, bSi, frb, ALU.mult, kb_ + ["BbS"], ["tqb"])
        tt(t_b, bSr, fib, ALU.mult, kb_, ["tqb"])
        tt(BbS[1][:], t_a, t_b, ALU.add, ["tqb"], ["BbS"])
        kg = ["G", "tqb", "bSr", "bSi"]
        dv(lambda e: e.memset(Gr[:, :, 127:128], 1.0), kg, kg)
        dv(lambda e: e.memset(Gi[:, :, 127:128], 0.0), kg, kg)
        dv(lambda e: e.tensor_copy(out=am[0][:], in_=aS[0][:]), ["aS"], kg)
        dv(lambda e: e.tensor_copy(out=am[1][:], in_=aS[1][:]), ["aS"], kg)
        m = 1
        while m < 128:
            src_r = Gr[:, :, 128 - m:128]
            src_i = Gi[:, :, 128 - m:128]
            amr = am[0][:].unsqueeze(2).to_broadcast([128, 32, m])
            ami = am[1][:].unsqueeze(2).to_broadcast([128, 32, m])
            tt(tq[0][:, :, 0:m], src_r, amr, ALU.mult, kg, kg)
            tt(tq[1][:, :, 0:m], src_i, ami, ALU.mult, kg, kg)
            tt(Gr[:, :, 128 - 2 * m:128 - m], tq[0][:, :, 0:m], tq[1][:, :, 0:m], ALU.subtract, kg, kg)
            tt(tq[0][:, :, 0:m], src_r, ami, ALU.mult, kg, kg)
            tt(tq[1][:, :, 0:m], src_i, amr, ALU.mult, kg, kg)
            tt(Gi[:, :, 128 - 2 * m:128 - m], tq[0][:, :, 0:m], tq[1][:, :, 0:m], ALU.add, kg, kg)
            tt(ts3[0][:], am[0][:], am[0][:], ALU.mult, kg, kg)
            tt(ts3[1][:], am[1][:], am[1][:], ALU.mult, kg, kg)
            tt(ts3[2][:], am[0][:], am[1][:], ALU.mult, kg, kg)
            tt(am[0][:], ts3[0][:], ts3[1][:], ALU.subtract, kg, kg)
            dv(lambda e: e.tensor_scalar(out=am[1][:], in0=ts3[2][:], scalar1=2.0, scalar2=None, op0=ALU.mult), kg, kg)
            m *= 2
        for x_, Gsrc in enumerate((Gr, Gi)):
            for q4 in range(8):
                pst = qs_m[q4 % 2]
                for i4 in range(4):
                    pi = q4 * 4 + i4
                    p.op("pe", lambda e, pi=pi, i4=i4, Gsrc=Gsrc, pst=pst: e.transpose(out=pst[:, i4 // 4, (i4 % 4) * 128:(i4 % 4 + 1) * 128], in_=Gsrc[:, pi, :], identity=identf[:]),
                         reads=["G", "identf"], writes=["qs_m%d" % (q4 % 2)])
                p.op("act", lambda e, x_=x_, q4=q4, pst=pst: e.activation(out=GT[x_][:, q4 * 4:q4 * 4 + 4, :].rearrange("p a s -> p (a s)"), in_=pst[:, 0, :], func=AF.Copy),
                     reads=["qs_m%d" % (q4 % 2)], writes=["GT"])
        p.fence()
        p.flush()
        Q2.close()
        xa = [p.sb(Q, "xa%d" % i, [128, 1024], F32) for i in range(2)]
        junka = p.sb(Q, "junka", [128, 1024], BF16)
        ssa = [p.sb(Q, "ssa%d" % i, [128, 1], F32) for i in range(2)]
        rstda = [p.sb(Q, "rstda%d" % i, [128, 1], F32) for i in range(2)]
        xsa = [p.sb(Q, "xsa%d" % i, [128, 1024], BF16) for i in range(2)]
        hna = [p.sb(Q, "hna%d" % i, [128, 8, 128], BF16) for i in range(2)]
        utk = [p.sb(Q, "utk%d" % i, [128, 1024], BF16) for i in range(2)]
        Ms = [[p.sb(Q, "Ms%d%d" % (x_, i), [128, 32, 32], F32) for i in range(2)] for x_ in range(2)]
        pw = [p.sb(Q, "pw%d" % i, [128, 32, 32], F32) for i in range(4)]
        Vt = [p.sb(Q, "Vt%d" % x_, [128, 32], F32) for x_ in range(2)]
        t8 = [p.sb(Q, "t8%d" % i, [128, 32], F32) for i in range(4)]

        def ctx_pro1(lb):
            bb = lb % 2
            sx = "%d" % bb
            p.dma("sp", xa[bb][:], D["xloc"][lb * 128:(lb + 1) * 128, :], writes=["xa" + sx])
            p.op("act", lambda e, bb=bb: e.activation(out=junka[:], in_=xa[bb][:], func=AF.Square, accum_out=ssa[bb][:]), reads=["xa" + sx], writes=["junka", "ssa" + sx])
            p.op("act", lambda e, bb=bb: e.activation(out=rstda[bb][:], in_=ssa[bb][:], func=AF.Sqrt, scale=1.0 / 1024, bias=EPS), reads=["ssa" + sx], writes=["rstda" + sx])
            dv(lambda e, bb=bb: e.reciprocal(out=rstda[bb][:], in_=rstda[bb][:]), ["rstda" + sx], ["rstda" + sx])
            p.op("act", lambda e, bb=bb: e.activation(out=xsa[bb][:], in_=xa[bb][:], func=AF.Copy, scale=rstda[bb][:, 0:1]), reads=["xa" + sx, "rstda" + sx], writes=["xsa" + sx])
            for kc in range(8):
                p.op("pe", lambda e, kc=kc, bb=bb: e.transpose(out=qs_tr[:, kc * 128:(kc + 1) * 128], in_=xsa[bb][:, kc * 128:(kc + 1) * 128], identity=ident[:]),
                     reads=["xsa" + sx, "ident"], writes=["qs_tr"])

        def ctx_pro2(lb):
            bb = lb % 2
            sx = "%d" % bb
            tt(hna[bb][:], qs_tr[:].rearrange("p (k t) -> p k t", k=8), g_mix[:].unsqueeze(2).to_broadcast([128, 8, 128]), ALU.mult,
               ["qs_tr", "g_mix"], ["hna" + sx])
            for hf in range(2):
                for kc in range(8):
                    p.op("pe", lambda e, hf=hf, kc=kc, bb=bb: e.matmul(qs_u[:, hf, :], lhsT=hna[bb][:, kc, :], rhs=w_u0[:, kc, hf * 512:(hf + 1) * 512],
                                                                       start=(kc == 0), stop=(kc == 7)), reads=["w_u0", "hna" + sx], writes=["qs_u"])
            p.op("act", lambda e, bb=bb: e.activation(out=utk[bb][:], in_=qs_u[:].rearrange("p a t -> p (a t)"), func=AF.Copy), reads=["qs_u"], writes=["utk" + sx])

        def ctx_pro3(lb):
            bb = lb % 2
            sx = "%d" % bb
            for x_ in range(2):
                for pi in range(32):
                    p.op("pe", lambda e, x_=x_, pi=pi, bb=bb: e.matmul(qs_m[x_][:, pi // 16, (pi % 16) * 32:(pi % 16) * 32 + 32], lhsT=GT[x_][:, pi, :],
                                                                       rhs=utk[bb][:, 32 * pi:32 * pi + 32], start=True, stop=True),
                         reads=["GT", "utk" + sx], writes=["qs_m%d" % x_])
                p.op("act", lambda e, x_=x_, bb=bb: e.activation(out=Ms[x_][bb][:].rearrange("p a c -> p (a c)"), in_=qs_m[x_][:].rearrange("p a t -> p (a t)"), func=AF.Copy),
                     reads=["qs_m%d" % x_], writes=["Ms%d" % x_ + sx])

        for i0 in range(0, NE * CAP + 128, 128):
            p.dma("pool", Xs_d[i0:i0 + 128, :], zrow[:], reads=["zrow"], writes=["Xs_d"], stream="xsz")
        ctx_pro1(0)
        ctx_pro2(0)
        ctx_pro3(0)
        for lb in range(NB_CTX - 1):
            bb = lb % 2
            sx = "%d" % bb
            if lb + 1 < NB_CTX - 1:
                ctx_pro1(lb + 1)
                ctx_pro2(lb + 1)
                ctx_pro3(lb + 1)
            Mr, Mi = Ms[0][bb], Ms[1][bb]
            kr, ki = "Ms0" + sx, "Ms1" + sx
            tt(pw[0][:], Mr[:], BbS[0][:], ALU.mult, [kr, "BbS"], ["pw0"])
            tt(pw[1][:], Mi[:], BbS[1][:], ALU.mult, [ki, "BbS"], ["pw1"])
            tt(pw[2][:], Mi[:], BbS[0][:], ALU.mult, [ki, "BbS"], ["pw2"])
            tt(pw[3][:], Mr[:], BbS[1][:], ALU.mult, [kr, "BbS"], ["pw3"])
            tt(pw[0][:], pw[0][:], pw[1][:], ALU.subtract, ["pw0", "pw1"], ["pw0"])
            tt(pw[2][:], pw[2][:], pw[3][:], ALU.add, ["pw2", "pw3"], ["pw2"])
            dv(lambda e: e.reduce_sum(out=Vt[0][:], in_=pw[0][:], axis=AX.X), ["pw0"], ["Vt0"])
            dv(lambda e: e.reduce_sum(out=Vt[1][:], in_=pw[2][:], axis=AX.X), ["pw2"], ["Vt1"])
            tt(t8[0][:], am[0][:], Xc[0][:], ALU.mult, ["G", "Xc"], ["t80"])
            tt(t8[1][:], am[1][:], Xc[1][:], ALU.mult, ["G", "Xc"], ["t81"])
            tt(t8[2][:], am[0][:], Xc[1][:], ALU.mult, ["G", "Xc"], ["t82"])
            tt(t8[3][:], am[1][:], Xc[0][:], ALU.mult, ["G", "Xc"], ["t83"])
            tt(t8[0][:], t8[0][:], t8[1][:], ALU.subtract, ["t80", "t81"], ["t80"])
            tt(t8[2][:], t8[2][:], t8[3][:], ALU.add, ["t82", "t83"], ["t82"])
            tt(Xc[0][:], t8[0][:], Vt[0][:], ALU.add, ["t80", "Vt0", "Xc"], ["Xc"])
            tt(Xc[1][:], t8[2][:], Vt[1][:], ALU.add, ["t82", "Vt1", "Xc"], ["Xc"])
        p.fence()
        p.flush()

    if stop == "tables":
        p.fence(); p.flush(); return nc
    xin = [p.sb(P1, "xin0", [128, 1024], F32)] * 2
    junk = p.sb(P1, "junk", [128, 1024], BF16)
    ss = p.sb(P1, "ss", [128, 1], F32)
    rstd = p.sb(P1, "rstd", [128, 1], F32)
    xsL = [p.sb(P1, "xs0", [128, 1024], BF16)] * 2
    hnL = [p.sb(P1, "hnT%d" % i, [128, 8, 128], BF16) for i in range(2)]
    uTbL = [p.sb(P1, "uTb%d" % i, [128, NJ, 128], BF16) for i in range(2)]
    uTfL = uTbL
    wk = [p.sb(P1, "wk%d" % i, [128, 8, 128], F32) for i in range(4)]
    xr = [p.sb(P1, "xr_r", [128, 32, 128], BF16), p.sb(P1, "xr_i", [128, 32, 128], BF16)]
    ysb = p.sb(P1, "ysb", [128, NJ, 128], F32)
    zTb = p.sb(P1, "zTb", [128, NJ, 128], BF16)
    qkf = p.sb(P1, "qkf", [128, 18, 64], F32)
    rc = p.sb(P1, "rc", [128, 8], F32)
    rs = p.sb(P1, "rs", [128, 8], F32)
    rt = [p.sb(P1, "rt%d" % i, [128, 18, 8], F32) for i in range(4)]
    qkb = p.sb(P1, "qkb", [128, 18, 64], BF16)
    qT = p.sb(P1, "qT", [64, 16, 128], BF16)
    kT = [p.sb(P1, "kT%d" % i, [64, 2, 128], BF16) for i in range(3)]
    vA = [p.sb(P1, "vA%d" % i, [128, 2, 65], BF16) for i in range(3)]
    for i in range(3):
        p.op("pool", lambda e, i=i: e.memset(vA[i][:], 1.0), writes=["vA%d" % i])
    Et = [p.sb(P1, "Et%d" % i, [128, 512], BF16) for i in range(4)]
    Etm = [p.sb(P1, "Etm%d" % i, [128, 512], BF16) for i in range(4)]
    Em = [p.sb(P1, "Em%d" % i, [16, 512], BF16) for i in range(4)]
    den = p.sb(P1, "den", [128, 16], F32)
    attn = p.sb(P1, "attn", [128, 16, 64], BF16)
    aT = p.sb(P1, "aT", [128, 8, 128], BF16)

    ps_tr = p.ps(P1, "ps_tr", [128, 1024], BF16)
    ps_u = p.ps(P1, "ps_u", [128, 3, 512], F32)
    ps_b = [p.ps(P1, "ps_b%d" % i, [128, 2, 512], F32) for i in range(2)]

    def norm_block(src, par):
        xt = xin[par]
        xk = "xin0"
        xs = xsL[par]
        hnT = hnL[par]
        kxs = "xs0"
        khn = "hnT%d" % par
        p.dma("sp", xt[:], src, writes=[xk])
        p.op("act", lambda e: e.activation(out=junk[:], in_=xt[:], func=AF.Square, accum_out=ss[:]), reads=[xk], writes=["junk", "ss"])
        p.op("act", lambda e: e.activation(out=rstd[:], in_=ss[:], func=AF.Sqrt, scale=1.0 / 1024, bias=EPS), reads=["ss"], writes=["rstd"])
        dv(lambda e: e.reciprocal(out=rstd[:], in_=rstd[:]), ["rstd"], ["rstd"])
        dv(lambda e: e.tensor_scalar(out=xs[:], in0=xt[:], scalar1=rstd[:, 0:1], scalar2=None, op0=ALU.mult), [xk, "rstd"], [kxs])
        for kc in range(8):
            p.op("pe", lambda e, kc=kc: e.transpose(out=ps_tr[:, kc * 128:(kc + 1) * 128], in_=xs[:, kc * 128:(kc + 1) * 128], identity=ident[:]),
                 reads=[kxs, "ident"], writes=["ps_tr"])
        tt(hnT[:], ps_tr[:].rearrange("p (k t) -> p k t", k=8), g_mix[:].unsqueeze(2).to_broadcast([128, 8, 128]), ALU.mult,
           ["ps_tr", "g_mix"], [khn])

    def uproj(par):
        hnT = hnL[par]
        uTf = uTfL[par]
        uTb = uTbL[par]
        khn = "hnT%d" % par
        kuf = "uTb%d" % par
        kub = "uTb%d" % par
        for j in range(NJ):
            for kc in range(8):
                p.op("pe", lambda e, j=j, kc=kc: e.matmul(ps_u[:, j // 4, (j % 4) * 128:(j % 4 + 1) * 128], lhsT=w_u[:, kc, j * 128:(j + 1) * 128],
                                                          rhs=hnT[:, kc, :], start=(kc == 0), stop=(kc == 7)),
                     reads=["w_u", khn], writes=["ps_u"])
        for b3 in range(3):
            n = 4 if b3 < 2 else 3
            p.op("act", lambda e, b3=b3, n=n: e.activation(out=uTb[:, b3 * 4:b3 * 4 + n, :], in_=ps_u[:, b3, 0:n * 128].rearrange("p (j t) -> p j t", j=n), func=AF.Copy),
                 reads=["ps_u"], writes=[kub])

    W127 = [p.sb(P1, "W127r", [128, 32], F32), p.sb(P1, "W127i", [128, 32], F32)]
    c6 = [p.sb(P1, "c6%d" % i, [128, 32], F32) for i in range(4)]

    def ssm_block(own, par):
        A, B, Cc, Dd = wk
        uTf = uTfL[par]
        uTb = uTbL[par]
        kuf = "uTb%d" % par
        kub = "uTb%d" % par
        for g8 in range(4):
            pb = ps_b
            for x_ in range(2):
                for pl8 in range(8):
                    pi = g8 * 8 + pl8
                    j, pl = divmod(pi, 3)
                    p.op("pe", lambda e, x_=x_, pl8=pl8, j=j, pl=pl: e.matmul(
                        pb[x_][:, pl8 // 4, (pl8 % 4) * 128:(pl8 % 4 + 1) * 128], lhsT=BbP[x_][:, j, pl, :],
                        rhs=uTb[:, j, :], start=True, stop=True), reads=["BbP", kub], writes=["ps_b%d" % x_])
            sl = slice(g8 * 8, g8 * 8 + 8)
            kA = [("wA", i) for i in range(8)]
            kB = [("wB", i) for i in range(8)]
            kC = [("wC", i) for i in range(8)]
            kD = [("wD", i) for i in range(8)]
            for hb in range(2):
                s4 = slice(g8 * 8 + hb * 4, g8 * 8 + hb * 4 + 4)
                h4 = slice(hb * 4, hb * 4 + 4)
                k4 = slice(hb * 4, hb * 4 + 4)
                br4 = pb[0][:, hb, :].rearrange("p (b t) -> p b t", b=4)
                bi4 = pb[1][:, hb, :].rearrange("p (b t) -> p b t", b=4)
                tt(A[:, h4, :], br4, Ec[:, s4, :], ALU.mult, ["E", "ps_b0"], kA[k4])
                tt(Cc[:, h4, :], bi4, Es[:, s4, :], ALU.mult, ["E", "ps_b1"], kC[k4])
                tt(B[:, h4, :], bi4, Ec[:, s4, :], ALU.mult, ["E", "ps_b1"], kB[k4])
                tt(Dd[:, h4, :], br4, Es[:, s4, :], ALU.mult, ["E", "ps_b0"], kD[k4])
                tt(A[:, h4, :], A[:, h4, :], Cc[:, h4, :], ALU.add, kA[k4] + kC[k4], kA[k4])
                tt(B[:, h4, :], B[:, h4, :], Dd[:, h4, :], ALU.subtract, kB[k4] + kD[k4], kB[k4])
            for pl8 in range(8):
                pi = g8 * 8 + pl8
                dv(lambda e, pl8=pl8, pi=pi: e.tensor_tensor_scan(out=Cc[:, pl8, :], data0=Rt[:, pi:pi + 1].to_broadcast([128, 128]), data1=A[:, pl8, :],
                                                                  initial=Xc[0][:, pi:pi + 1], op0=ALU.mult, op1=ALU.add), [kA[pl8], "Rt", "Xc"], [kC[pl8]])
            for pl8 in range(8):
                pi = g8 * 8 + pl8
                dv(lambda e, pl8=pl8, pi=pi: e.tensor_tensor_scan(out=Dd[:, pl8, :], data0=Rt[:, pi:pi + 1].to_broadcast([128, 128]), data1=B[:, pl8, :],
                                                                  initial=Xc[1][:, pi:pi + 1], op0=ALU.mult, op1=ALU.add), [kB[pl8], "Rt", "Xc"], [kD[pl8]])
            if own:
                tt(A[:], Ec[:, sl, :], Cc[:], ALU.mult, kC + kA + ["E"], kA)
                tt(B[:], Es[:, sl, :], Dd[:], ALU.mult, kD + kB + ["E"], kB)
                tt(xr[0][:, sl, :], A[:], B[:], ALU.subtract, kA + kB, [("xr0", g8)])
            dv(lambda e, sl=sl: e.tensor_copy(out=W127[0][:, sl], in_=Cc[:, :, 127]), kC, ["W127r"])
            dv(lambda e, sl=sl: e.tensor_copy(out=W127[1][:, sl], in_=Dd[:, :, 127]), kD, ["W127i"])
            if own:
                tt(A[:], Ec[:, sl, :], Dd[:], ALU.mult, kD + kA + ["E"], kA)
                tt(B[:], Es[:, sl, :], Cc[:], ALU.mult, kC + kB + ["E"], kB)
                tt(xr[1][:, sl, :], A[:], B[:], ALU.add, kA + kB, [("xr1", g8)])
            yield
        e_c = Ec[:, :, 127]
        e_s = Es[:, :, 127]
        tt(c6[0][:], e_c, W127[0][:], ALU.mult, ["E", "W127r"], ["c60"])
        tt(c6[1][:], e_s, W127[1][:], ALU.mult, ["E", "W127i"], ["c61"])
        tt(c6[2][:], e_c, W127[1][:], ALU.mult, ["E", "W127i"], ["c62"])
        tt(c6[3][:], e_s, W127[0][:], ALU.mult, ["E", "W127r"], ["c63"])
        tt(Xc[0][:], c6[0][:], c6[1][:], ALU.subtract, ["c60", "c61", "Xc"], ["Xc"])
        tt(Xc[1][:], c6[2][:], c6[3][:], ALU.add, ["c62", "c63", "Xc"], ["Xc"])
        if own:
            kx = [("xr0", g) for g in range(4)] + [("xr1", g) for g in range(4)]
            for pi in range(32):
                j, pl = divmod(pi, 3)
                lastpl = 2 if j < NJ - 1 else 1
                for x_ in range(2):
                    p.op("pe", lambda e, pi=pi, j=j, pl=pl, x_=x_, lastpl=lastpl: e.matmul(
                        ps_u[:, j // 4, (j % 4) * 128:(j % 4 + 1) * 128], lhsT=CTP[x_][:, pi, :], rhs=xr[x_][:, pi, :],
                        start=(pl == 0 and x_ == 0), stop=(pl == lastpl and x_ == 1)), reads=["CTP", ("xr%d" % x_, pi // 8)], writes=["ps_u"])
            tt(ysb[:], uTf[:], d_pad[:].unsqueeze(2).to_broadcast([128, NJ, 128]), ALU.mult, [kuf, "d_pad"], ["ysb"])
            for b3 in range(3):
                n = 4 if b3 < 2 else 3
                tt(ysb[:, b3 * 4:b3 * 4 + n, :], ysb[:, b3 * 4:b3 * 4 + n, :], ps_u[:, b3, 0:n * 128].rearrange("p (j t) -> p j t", j=n), ALU.add, ["ysb", "ps_u"], ["ysb"])
            p.op("act", lambda e: e.activation(out=zTb[:], in_=ysb[:], func=AF.Gelu), reads=["ysb"], writes=["zTb"])

    def qkv_block(bidx, slot, par, meta=False):
        hnT = hnL[par]
        khn = "hnT%d" % par
        for c0, c1 in ((0, 512), (512, 1024), (1024, 1280)):
            for kc in range(8):
                p.op("pe", lambda e, c0=c0, c1=c1, kc=kc: e.matmul(ps_u[:, c0 // 512, 0:c1 - c0], lhsT=hnT[:, kc, :], rhs=w_qkv[:, kc, c0:c1],
                                                                   start=(kc == 0), stop=(kc == 7)), reads=[khn, "w_qkv"], writes=["ps_u"])
        p.dma("sp", rc[:], D["rcos"][bidx], writes=["rc"])
        p.dma("sp", rs[:], D["rsin"][bidx], writes=["rs"])
        p.op("act", lambda e: e.activation(out=qkf[:, 0:16, :].rearrange("p h d -> p (h d)"), in_=ps_u[:, 0:2, :].rearrange("p a b -> p (a b)"), func=AF.Copy),
             reads=["ps_u"], writes=["qkf"])
        p.op("act", lambda e: e.activation(out=qkf[:, 16:18, :].rearrange("p h d -> p (h d)"), in_=ps_u[:, 2, 0:128], func=AF.Copy),
             reads=["ps_u"], writes=["qkf"])
        vk = "vA%d" % slot
        p.op("act", lambda e: e.activation(out=vA[slot][:, :, 0:64], in_=ps_u[:, 2, 128:256].rearrange("p (h d) -> p h d", h=2), func=AF.Copy),
             reads=["ps_u"], writes=[vk])
        p.op("act", lambda e: e.activation(out=qkb[:].rearrange("p h d -> p (h d)"), in_=qkf[:].rearrange("p h d -> p (h d)"), func=AF.Copy), reads=["qkf"], writes=["qkb"])
        cb = rc[:].unsqueeze(1).to_broadcast([128, 18, 8])
        sbb = rs[:].unsqueeze(1).to_broadcast([128, 18, 8])
        x1 = qkf[:, :, 0:8]
        x2 = qkf[:, :, 8:16]
        kr = ["qkf", "rc", "rs", "rt"]
        tt(rt[0][:], x1, cb, ALU.mult, kr, ["rt"])
        tt(rt[1][:], x2, sbb, ALU.mult, kr, ["rt"])
        tt(qkb[:, :, 0:8], rt[0][:], rt[1][:], ALU.subtract, ["rt", "qkb"], ["qkb"])
        tt(rt[2][:], x2, cb, ALU.mult, kr, ["rt"])
        tt(rt[3][:], x1, sbb, ALU.mult, kr, ["rt"])
        tt(qkb[:, :, 8:16], rt[2][:], rt[3][:], ALU.add, ["rt", "qkb"], ["qkb"])
        kk = "kT%d" % slot
        for h in range(2):
            p.op("pe", lambda e, h=h: e.transpose(out=ps_tr[0:64, h * 128:(h + 1) * 128], in_=qkb[:, 16 + h, :], identity=ident[:]),
                 reads=["qkb", "ident"], writes=["ps_tr"])
        dv(lambda e: e.tensor_copy(out=kT[slot][:].rearrange("p h t -> p (h t)"), in_=ps_tr[0:64, 0:256]), ["ps_tr"], [kk])
        if not meta:
            for half in range(2):
                for h in range(8):
                    p.op("pe", lambda e, h=h, half=half: e.transpose(out=ps_tr[0:64, h * 128:(h + 1) * 128], in_=qkb[:, half * 8 + h, :], identity=ident[:]),
                         reads=["qkb", "ident"], writes=["ps_tr"])
                dv(lambda e, half=half: e.tensor_copy(out=qT[:, half * 8:half * 8 + 8, :].rearrange("p h t -> p (h t)"), in_=ps_tr[0:64, :]), ["ps_tr"], ["qT"])

    scale = 1.0 / math.sqrt(64.0)

    def attn_block(ob, cur, prev, first):
        mp = m_first if first else m_prev
        for kap in range(2):
            for half in range(2):
                rhs = qT[:, kap * 8 + half * 4:kap * 8 + half * 4 + 4, :].rearrange("p h t -> p (h t)")
                i = kap * 2 + half
                pbank = ps_b[half]
                p.op("pe", lambda e, kap=kap, rhs=rhs, pbank=pbank: e.matmul(pbank[:, 0, :], lhsT=kT[prev][:, kap, :], rhs=rhs, start=True, stop=True),
                     reads=["kT%d" % prev, "qT"], writes=["ps_b%d" % half])
                p.op("pe", lambda e, kap=kap, rhs=rhs, pbank=pbank: e.matmul(pbank[:, 1, :], lhsT=kT[cur][:, kap, :], rhs=rhs, start=True, stop=True),
                     reads=["kT%d" % cur, "qT"], writes=["ps_b%d" % half])
                ek = "Et%d" % i
                p.op("act", lambda e, i=i, pbank=pbank: e.activation(out=Et[i][:], in_=pbank[:, 0, :], func=AF.Exp, scale=scale), reads=["ps_b%d" % half], writes=[ek])
                p.op("act", lambda e, i=i, pbank=pbank: e.activation(out=Etm[i][:], in_=pbank[:, 1, :], func=AF.Exp, scale=scale), reads=["ps_b%d" % half], writes=[ek + "c"])
                tt(Et[i][:].rearrange("p (h t) -> p h t", h=4), Et[i][:].rearrange("p (h t) -> p h t", h=4), mp[:].unsqueeze(1).to_broadcast([128, 4, 128]),
                   ALU.mult, [ek, "m_first", "m_prev"], [ek])
                tt(Etm[i][:].rearrange("p (h t) -> p h t", h=4), Etm[i][:].rearrange("p (h t) -> p h t", h=4), m_cur[:].unsqueeze(1).to_broadcast([128, 4, 128]),
                   ALU.mult, [ek + "c", "m_cur"], [ek + "c"])
                p.op("pe", lambda e, kap=kap, rhs=rhs: e.matmul(ps_u[0:16, 2, :], lhsT=kT[2][:, kap, 0:16], rhs=rhs, start=True, stop=True),
                     reads=["kT2", "qT"], writes=["ps_u"])
                p.op("act", lambda e, i=i: e.activation(out=Em[i][:], in_=ps_u[0:16, 2, :], func=AF.Exp, scale=scale), reads=["ps_u"], writes=["Em%d" % i])
            yield
        for h in range(16):
            kap = h // 8
            i = kap * 2 + (h % 8) // 4
            c = (h % 4) * 128
            o = ps_u[:, h // 7, (h % 7) * 65:(h % 7) * 65 + 65]
            p.op("pe", lambda e, i=i, c=c, o=o, kap=kap: e.matmul(o, lhsT=Et[i][:, c:c + 128], rhs=vA[prev][:, kap, :], start=True, stop=False),
                 reads=["Et%d" % i, "vA%d" % prev], writes=["ps_u"])
            p.op("pe", lambda e, i=i, c=c, o=o, kap=kap: e.matmul(o, lhsT=Etm[i][:, c:c + 128], rhs=vA[cur][:, kap, :], start=False, stop=False),
                 reads=["Et%dc" % i, "vA%d" % cur], writes=["ps_u"])
            p.op("pe", lambda e, i=i, c=c, o=o, kap=kap: e.matmul(o, lhsT=Em[i][:, c:c + 128], rhs=vA[2][0:16, kap, :], start=False, stop=True),
                 reads=["Em%d" % i, "vA2"], writes=["ps_u"])
        for bk in range(3):
            n = 7 if bk < 2 else 2
            h0 = bk * 7
            pv = ps_u[:, bk, 0:n * 65].rearrange("p (h d) -> p h d", h=n)
            tt(den[:, h0:h0 + n], pv[:, :, 64], esink[:, h0:h0 + n], ALU.add, ["ps_u", "esink"], ["den"])
            dv(lambda e, h0=h0, n=n: e.reciprocal(out=den[:, h0:h0 + n], in_=den[:, h0:h0 + n]), ["den"], ["den"])
            tt(attn[:, h0:h0 + n, :], pv[:, :, 0:64], den[:, h0:h0 + n].unsqueeze(2).to_broadcast([128, n, 64]), ALU.mult, ["ps_u", "den"], ["attn"])
        yield
        for kc in range(8):
            p.op("pe", lambda e, kc=kc: e.transpose(out=ps_tr[:, kc * 128:(kc + 1) * 128], in_=attn[:, 2 * kc:2 * kc + 2, :].rearrange("p h d -> p (h d)"), identity=ident[:]),
                 reads=["attn", "ident"], writes=["ps_tr"])
        dv(lambda e: e.tensor_copy(out=aT[:].rearrange("p k t -> p (k t)"), in_=ps_tr[:]), ["ps_tr"], ["aT"])
        p.dma("sp", aT_d[ob], aT[:].rearrange("p k t -> p (k t)"), reads=["aT"], writes=["aT_d"])
        yield


    norm_block(D["xmeta"], 1)
    qkv_block(0, 2, 1, meta=True)
    if stop == "meta":
        p.fence(); p.flush(); return nc
    blocks = list(range(NB_CTX - 1 if CTX_FAST else 0, NB))

    def pro_a(lb, par):
        norm_block(D["xloc"][lb * 128:(lb + 1) * 128, :], par)
        uproj(par)

    def pro_b(lb, par):
        if lb >= NB_CTX - 1:
            qkv_block(lb + 1, lb % 2, par, meta=False)
    pro_a(blocks[0], 0)
    pro_b(blocks[0], 0)
    for bi, lb in enumerate(blocks):
        par = bi % 2
        own = lb >= NB_CTX
        nxt = blocks[bi + 1] if bi + 1 < len(blocks) else None
        gs = ssm_block(own, par)
        ga = attn_block(lb - NB_CTX, lb % 2, (lb - 1) % 2, lb == NB_CTX) if own else iter(())
        for st in range(4):
            next(gs)
            next(ga, None)
            if st == 0 and nxt is not None:
                pro_a(nxt, 1 - par)
        for _ in gs:
            pass
        for _ in ga:
            pass
        if own:
            ob = lb - NB_CTX
            p.dma("sp", zT_d[ob], zTb[:].rearrange("p j t -> p (j t)"), reads=["zTb"], writes=["zT_d"])
            p.dma("sp", hT_d[ob], hnL[par][:].rearrange("p k t -> p (k t)"), reads=["hnT%d" % par], writes=["hT_d"])
        if nxt is not None:
            pro_b(nxt, 1 - par)
    p.fence()
    p.flush()
    if stop == "p1":
        return nc
    P1.close()

    P2 = ExitStack()
    w_g = load(P2, "w_g", [128, 8, 2048], BF16, D["w_g"], "pool")
    w_glu = load(P2, "w_glu", [128, NJ, NJ * 128], BF16, D["w_glu"], "pool")
    w_brs = load(P2, "w_brs", [128, NJ, 1024], BF16, D["w_brs"], "pool")
    w_bra = load(P2, "w_bra", [128, 8, 1024], BF16, D["w_bra"], "pool")
    w_out = load(P2, "w_out", [128, 8, 1024], BF16, D["w_out"], "pool")
    w_rt = load(P2, "w_rt", [128, 8, 32], F32, D["w_rt"])
    b_glu = load(P2, "b_glu", [128, NJ], F32, D["b_glu"])
    b_rt = load(P2, "b_rt", [128, 32], F32, D["b_rt"])
    g_ffn = load(P2, "g_ffn", [128, 1024], F32, D["g_ffn"])
    tri = load(P2, "tri", [128, 128], BF16, D["tri"], "pool")
    ones = p.sb(P2, "ones", [128, 128], BF16)
    p.op("pool", lambda e: e.memset(ones[:], 1.0), writes=["ones"])
    iota_i = p.sb(P2, "iota_i", [128, 32], I32)
    iota_f = p.sb(P2, "iota_f", [128, 32], F32)
    p.op("pool", lambda e: e.iota(iota_i[:], pattern=[[1, 32]], base=0, channel_multiplier=0), writes=["iota_i"])
    dv(lambda e: e.tensor_copy(out=iota_f[:], in_=iota_i[:]), ["iota_i"], ["iota_f"])
    base = p.sb(P2, "base", [128, 32], F32)
    dv(lambda e: e.memset(base[:], 0.0), [], ["base"])

    hT2L = [p.sb(P2, "hT2_%d" % i, [128, 8, 128], BF16) for i in range(2)]
    zT2L = [p.sb(P2, "zT2_%d" % i, [128, NJ, 128], BF16) for i in range(2)]
    aT2L = [p.sb(P2, "aT2_%d" % i, [128, 8, 128], BF16) for i in range(2)]
    x2L = [p.sb(P2, "x2_%d" % i, [128, 1024], F32) for i in range(2)]
    sg = p.sb(P2, "sg", [128, 16, 128], BF16)
    sig = p.sb(P2, "sig", [128, NJ, 128], BF16)
    so = p.sb(P2, "so", [128, NJ, 128], BF16)
    mixa = p.sb(P2, "mixa", [128, 8, 128], F32)
    mixb = p.sb(P2, "mixb", [128, 8, 128], F32)
    mixT = p.sb(P2, "mixT", [128, 8, 128], BF16)
    h2 = p.sb(P2, "h2", [128, 1024], F32)
    junk2 = p.sb(P2, "junk2", [128, 1024], F32)
    ss2 = p.sb(P2, "ss2", [128, 1], F32)
    rstd2 = p.sb(P2, "rstd2", [128, 1], F32)
    xn = p.sb(P2, "xn", [128, 1024], F32)
    xnb = p.sb(P2, "xnb", [128, 1024], BF16)
    xnT = p.sb(P2, "xnT", [128, 8, 128], F32)
    lg = p.sb(P2, "lg", [128, 32], F32)
    top8 = p.sb(P2, "top8", [128, 8], F32)
    idx8 = p.sb(P2, "idx8", [128, 8], U32)
    ex4 = p.sb(P2, "ex4", [128, 4], F32)
    nmx = p.sb(P2, "nmx", [128, 1], F32)
    sm4 = p.sb(P2, "sm4", [128, 1], F32)
    Mb = p.sb(P2, "Mb", [128, 32], BF16)
    pos = p.sb(P2, "pos", [128, 32], F32)
    oh = p.sb(P2, "oh", [128, 32], F32)
    sp4 = p.sb(P2, "sp4", [128, 4], F32)
    slf = p.sb(P2, "slf", [128, 4], F32)
    pq = [p.ps(P2, "pq%d" % i, [128, 512], F32) for i in range(8)]

    for ob in range(NB_OWN):
        hT2, zT2, aT2, x2 = hT2L[ob % 2], zT2L[ob % 2], aT2L[ob % 2], x2L[ob % 2]
        khT, kzT, kaT, kx2 = "hT2_%d" % (ob % 2), "zT2_%d" % (ob % 2), "aT2_%d" % (ob % 2), "x2_%d" % (ob % 2)
        p.dma("sp", hT2[:].rearrange("p k t -> p (k t)"), hT_d[ob], reads=["hT_d"], writes=[khT])
        p.dma("sp", zT2[:].rearrange("p j t -> p (j t)"), zT_d[ob], reads=["zT_d"], writes=[kzT])
        p.dma("sp", aT2[:].rearrange("p k t -> p (k t)"), aT_d[ob], reads=["aT_d"], writes=[kaT])
        p.dma("sp", x2[:], D["xloc"][(NB_CTX + ob) * 128:(NB_CTX + ob + 1) * 128, :], writes=[kx2])
        for oc in range(16):
            pt = pq[oc // 4]
            for kc in range(8):
                p.op("pe", lambda e, oc=oc, kc=kc, pt=pt, hT2=hT2: e.matmul(pt[:, (oc % 4) * 128:(oc % 4 + 1) * 128], lhsT=w_g[:, kc, oc * 128:(oc + 1) * 128], rhs=hT2[:, kc, :],
                                                                   start=(kc == 0), stop=(kc == 7)), reads=["w_g", khT], writes=["pq%d" % (oc // 4)])
        for b4 in range(4):
            p.op("act", lambda e, b4=b4: e.activation(out=sg[:, b4 * 4:b4 * 4 + 4, :].rearrange("p a t -> p (a t)"), in_=pq[b4][:], func=AF.Sigmoid),
                 reads=["pq%d" % b4], writes=["sg"])
        for oc in range(NJ):
            pt = pq[4 + oc // 4]
            for kc in range(NJ):
                p.op("pe", lambda e, oc=oc, kc=kc, pt=pt, zT2=zT2: e.matmul(pt[:, (oc % 4) * 128:(oc % 4 + 1) * 128], lhsT=w_glu[:, kc, oc * 128:(oc + 1) * 128], rhs=zT2[:, kc, :],
                                                                   start=(kc == 0), stop=(kc == NJ - 1)), reads=["w_glu", kzT], writes=["pq%d" % (4 + oc // 4)])
        for oc in range(NJ):
            p.op("act", lambda e, oc=oc: e.activation(out=sig[:, oc, :], in_=pq[4 + oc // 4][:, (oc % 4) * 128:(oc % 4 + 1) * 128], func=AF.Sigmoid, bias=b_glu[:, oc:oc + 1]),
                 reads=["pq%d" % (4 + oc // 4), "b_glu"], writes=["sig"])
        tt(so[:], zT2[:], sig[:], ALU.mult, [kzT, "sig"], ["so"])
        for oc in range(8):
            pt = pq[oc // 4]
            for kc in range(NJ):
                p.op("pe", lambda e, oc=oc, kc=kc, pt=pt: e.matmul(pt[:, (oc % 4) * 128:(oc % 4 + 1) * 128], lhsT=w_brs[:, kc, oc * 128:(oc + 1) * 128], rhs=so[:, kc, :],
                                                                   start=(kc == 0), stop=(kc == NJ - 1)), reads=["w_brs", "so"], writes=["pq%d" % (oc // 4)])
            pt2 = pq[2 + oc // 4]
            for kc in range(8):
                p.op("pe", lambda e, oc=oc, kc=kc, pt2=pt2, aT2=aT2: e.matmul(pt2[:, (oc % 4) * 128:(oc % 4 + 1) * 128], lhsT=w_bra[:, kc, oc * 128:(oc + 1) * 128], rhs=aT2[:, kc, :],
                                                                     start=(kc == 0), stop=(kc == 7)), reads=["w_bra", kaT], writes=["pq%d" % (2 + oc // 4)])
        for b2 in range(2):
            tt(mixa[:, b2 * 4:b2 * 4 + 4, :].rearrange("p a t -> p (a t)"), pq[b2][:], sg[:, b2 * 4:b2 * 4 + 4, :].rearrange("p a t -> p (a t)"), ALU.mult,
               ["pq%d" % b2, "sg"], ["mixa"])
            tt(mixb[:, b2 * 4:b2 * 4 + 4, :].rearrange("p a t -> p (a t)"), pq[2 + b2][:], sg[:, 8 + b2 * 4:8 + b2 * 4 + 4, :].rearrange("p a t -> p (a t)"), ALU.mult,
               ["pq%d" % (2 + b2), "sg"], ["mixb"])
        tt(mixT[:], mixa[:], mixb[:], ALU.add, ["mixa", "mixb"], ["mixT"])
        for hf in range(2):
            for kc in range(8):
                p.op("pe", lambda e, hf=hf, kc=kc: e.matmul(pq[4 + hf][:], lhsT=mixT[:, kc, :], rhs=w_out[:, kc, hf * 512:(hf + 1) * 512], start=(kc == 0), stop=(kc == 7)),
                     reads=["mixT", "w_out"], writes=["pq%d" % (4 + hf)])
            tt(h2[:, hf * 512:(hf + 1) * 512], pq[4 + hf][:], x2[:, hf * 512:(hf + 1) * 512], ALU.add, ["pq%d" % (4 + hf), kx2], ["h2"])
        p.dma("sp", h2_d[ob * 128:(ob + 1) * 128, :], h2[:], reads=["h2"], writes=["h2_d"])
        if debug:
            p.dma("sp", dbg["h2"][ob * 128:(ob + 1) * 128, :], h2[:], reads=["h2"], writes=["dbg_h2"])
        p.op("act", lambda e: e.activation(out=junk2[:], in_=h2[:], func=AF.Square, accum_out=ss2[:]), reads=["h2"], writes=["junk2", "ss2"])
        p.op("act", lambda e: e.activation(out=rstd2[:], in_=ss2[:], func=AF.Sqrt, scale=1.0 / 1024, bias=EPS), reads=["ss2"], writes=["rstd2"])
        dv(lambda e: e.reciprocal(out=rstd2[:], in_=rstd2[:]), ["rstd2"], ["rstd2"])
        dv(lambda e: e.scalar_tensor_tensor(out=xn[:], in0=h2[:], scalar=rstd2[:, 0:1], in1=g_ffn[:], op0=ALU.mult, op1=ALU.mult), ["h2", "rstd2", "g_ffn"], ["xn"])
        dv(lambda e: e.tensor_copy(out=xnb[:], in_=xn[:]), ["xn"], ["xnb"], eng="pool")
        for kc in range(8):
            p.op("pe", lambda e, kc=kc: e.transpose(out=pq[6 + kc // 4][:, (kc % 4) * 128:(kc % 4 + 1) * 128], in_=xn[:, kc * 128:(kc + 1) * 128], identity=identf[:]),
                 reads=["xn", "identf"], writes=["pq%d" % (6 + kc // 4)])
        for b2 in range(2):
            p.op("act", lambda e, b2=b2: e.activation(out=xnT[:, b2 * 4:b2 * 4 + 4, :].rearrange("p a t -> p (a t)"), in_=pq[6 + b2][:], func=AF.Copy),
                 reads=["pq%d" % (6 + b2)], writes=["xnT"])
        for kc in range(8):
            p.op("pe", lambda e, kc=kc: e.matmul(pq[6][:, 0:32], lhsT=xnT[:, kc, :], rhs=w_rt[:, kc, :], start=(kc == 0), stop=(kc == 7)),
                 reads=["xnT", "w_rt"], writes=["pq6"])
        tt(lg[:], pq[6][:, 0:32], b_rt[:], ALU.add, ["pq6", "b_rt"], ["lg"])
        dv(lambda e: e.max(out=top8[:], in_=lg[:]), ["lg"], ["top8"])
        dv(lambda e: e.max_index(out=idx8[:], in_max=top8[:], in_values=lg[:]), ["lg", "top8"], ["idx8"])
        dv(lambda e, ob=ob: e.tensor_copy(out=idx_all[:, ob, :], in_=idx8[:, 0:4]), ["idx8"], ["idx_all"])
        dv(lambda e: e.tensor_scalar(out=nmx[:], in0=top8[:, 0:1], scalar1=-1.0, scalar2=None, op0=ALU.mult), ["top8"], ["nmx"])
        p.op("act", lambda e: e.activation(out=ex4[:], in_=top8[:, 0:4], func=AF.Exp, bias=nmx[:, 0:1], accum_out=sm4[:]), reads=["top8", "nmx"], writes=["ex4", "sm4"])
        dv(lambda e: e.reciprocal(out=sm4[:], in_=sm4[:]), ["sm4"], ["sm4"])
        dv(lambda e, ob=ob: e.tensor_scalar(out=gate_all[:, ob, :], in0=ex4[:], scalar1=sm4[:, 0:1], scalar2=None, op0=ALU.mult), ["ex4", "sm4"], ["gate_all"])
        dv(lambda e: e.tensor_scalar(out=Mb[:], in0=lg[:], scalar1=top8[:, 3:4], scalar2=None, op0=ALU.is_ge), ["lg", "top8"], ["Mb"])
        p.op("pe", lambda e: e.matmul(pq[7][:, 0:32], lhsT=tri[:], rhs=Mb[:], start=True, stop=True), reads=["tri", "Mb"], writes=["pq7"])
        p.op("pe", lambda e: e.matmul(pq[7][:, 32:64], lhsT=ones[:], rhs=Mb[:], start=True, stop=True), reads=["ones", "Mb"], writes=["pq7"])
        tt(pos[:], pq[7][:, 0:32], base[:], ALU.add, ["pq7", "base"], ["pos"])
        tt(base[:], pq[7][:, 32:64], base[:], ALU.add, ["pq7", "base", "pos"], ["base"])
        for k4 in range(4):
            dv(lambda e, k4=k4, ob=ob: e.tensor_scalar(out=oh[:], in0=iota_f[:], scalar1=idx_all[:, ob, k4:k4 + 1], scalar2=None, op0=ALU.is_equal),
               ["iota_f", "idx_all"], ["oh"])
            tt(oh[:], oh[:], pos[:], ALU.mult, ["oh", "pos"], ["oh"])
            dv(lambda e, k4=k4: e.reduce_sum(out=sp4[:, k4:k4 + 1], in_=oh[:], axis=AX.X), ["oh"], ["sp4"])
        dv(lambda e, ob=ob: e.scalar_tensor_tensor(out=slf[:], in0=idx_all[:, ob, :], scalar=float(CAP), in1=sp4[:], op0=ALU.mult, op1=ALU.add),
           ["idx_all", "sp4"], ["slf"])
        dv(lambda e, ob=ob: e.tensor_copy(out=slot_all[:, ob, :], in_=slf[:]), ["slf"], ["slot_all"])
        for k4 in range(4):
            p.dma("pool", None, None, reads=["slot_all", "xnb"], writes=["Xs_d"], stream="scat",
                  fn=lambda e, ob=ob, k4=k4: e.indirect_dma_start(out=Xs_d, out_offset=bass.IndirectOffsetOnAxis(ap=slot_all[:, ob, k4:k4 + 1], axis=0),
                                                                   in_=xnb[:], in_offset=None))
    p.fence()
    p.flush()
    if stop == "p2":
        return nc
    P2.close()

    P3 = ExitStack()
    b_gu = load(P3, "b_gu", [128, 32, 16], F32, D["b_gu"])
    wgu = [p.sb(P3, "wgu%d" % i, [128, 8, 2048], BF16) for i in range(2)]
    wdn = [p.sb(P3, "wdn%d" % i, [128, 8, 1024], BF16) for i in range(2)]
    xe = [p.sb(P3, "xe%d" % i, [128, 1024], BF16) for i in range(3)]
    xeT = p.sb(P3, "xeT", [128, 8, CAP], BF16)
    hid = p.sb(P3, "hid", [128, 8, CAP], BF16)
    gc = p.sb(P3, "gc", [128, CAP], F32)
    sgm = p.sb(P3, "sgm", [128, CAP], F32)
    uc = p.sb(P3, "uc", [128, CAP], F32)
    ysl = [p.sb(P3, "ysl%d" % i, [128, 1024], F32) for i in range(2)]
    pg = [p.ps(P3, "pg%d" % i, [128, 512], F32) for i in range(4)]
    pd = [p.ps(P3, "pd%d" % i, [128, 512], F32) for i in range(2)]
    pt3 = p.ps(P3, "pt3", [128, 1024], BF16)
    NST = CAP // 128
    def load_w(ex):
        wi = ex % 2
        for kc in range(8):
            p.dma("pool", wgu[wi][:, kc, :], D["w_gu"][ex, kc * 128:(kc + 1) * 128, :], writes=["wgu%d" % wi], stream="wgu%d" % wi)
        p.dma("sp", wdf[:], D["w_dn"][ex].rearrange("(k p) n -> p k n", p=128), writes=["wdf"], stream="wdf")
    wdf = p.sb(P3, "wdf", [128, 8, 1024], F32)

    def cast_w(ex):
        wi = ex % 2
        p.op("act", lambda e, wi=wi: e.activation(out=wdn[wi][:].rearrange("p k n -> p (k n)"), in_=wdf[:].rearrange("p k n -> p (k n)"), func=AF.Copy),
             reads=["wdf"], writes=["wdn%d" % wi])
    xeT2 = [xeT, p.sb(P3, "xeTb", [128, 8, CAP], BF16)]

    def load_x(ex):
        xt = xeT2[ex % 2]
        for st_ in range(NST):
            p.dma("sp", xe[st_][:], Xs_d[ex * CAP + st_ * 128: ex * CAP + (st_ + 1) * 128, :], reads=["Xs_d"], writes=["xe%d" % st_])
            for kc in range(8):
                p.op("pe", lambda e, st_=st_, kc=kc: e.transpose(out=pt3[:, kc * 128:(kc + 1) * 128], in_=xe[st_][:, kc * 128:(kc + 1) * 128], identity=ident[:]),
                     reads=["xe%d" % st_, "ident"], writes=["pt3"])
            dv(lambda e, st_=st_, xt=xt: e.tensor_copy(out=xt[:, :, st_ * 128:(st_ + 1) * 128], in_=pt3[:].rearrange("p (k t) -> p k t", k=8)), ["pt3"], ["xeT%d" % (ex % 2)])
    load_w(0)
    cast_w(0)
    load_x(0)
    for ex in range(NE):
        wi = ex % 2
        xeT = xeT2[ex % 2]
        kxe = "xeT%d" % (ex % 2)
        if ex + 1 < NE:
            load_w(ex + 1)
        for fc in range(8):
            pgt = pg[(fc % 2) * 2]
            put = pg[(fc % 2) * 2 + 1]
            for kc in range(8):
                p.op("pe", lambda e, fc=fc, kc=kc, pgt=pgt, wi=wi, xeT=xeT: e.matmul(pgt[:, 0:CAP], lhsT=wgu[wi][:, kc, fc * 128:(fc + 1) * 128], rhs=xeT[:, kc, :], start=(kc == 0), stop=(kc == 7)),
                     reads=["wgu%d" % wi, kxe], writes=["pg%d" % ((fc % 2) * 2)])
            for kc in range(8):
                p.op("pe", lambda e, fc=fc, kc=kc, put=put, wi=wi, xeT=xeT: e.matmul(put[:, 0:CAP], lhsT=wgu[wi][:, kc, 1024 + fc * 128:1024 + (fc + 1) * 128], rhs=xeT[:, kc, :], start=(kc == 0), stop=(kc == 7)),
                     reads=["wgu%d" % wi, kxe], writes=["pg%d" % ((fc % 2) * 2 + 1)])
            dv(lambda e, ex=ex, fc=fc, pgt=pgt: e.tensor_scalar(out=gc[:], in0=pgt[:, 0:CAP], scalar1=b_gu[:, ex, fc:fc + 1], scalar2=7.0, op0=ALU.add, op1=ALU.min),
               ["pg%d" % ((fc % 2) * 2), "b_gu"], ["gc"])
            p.op("act", lambda e: e.activation(out=sgm[:], in_=gc[:], func=AF.Sigmoid, scale=1.702), reads=["gc"], writes=["sgm"])
            dv(lambda e, ex=ex, fc=fc, put=put: e.tensor_scalar(out=uc[:], in0=put[:, 0:CAP], scalar1=b_gu[:, ex, 8 + fc:9 + fc], scalar2=7.0, op0=ALU.add, op1=ALU.min),
               ["pg%d" % ((fc % 2) * 2 + 1), "b_gu"], ["uc"])
            dv(lambda e: e.tensor_scalar(out=uc[:], in0=uc[:], scalar1=-7.0, scalar2=1.0, op0=ALU.max, op1=ALU.add), ["uc"], ["uc"])
            tt(gc[:], gc[:], sgm[:], ALU.mult, ["gc", "sgm"], ["gc"])
            tt(hid[:, fc, :], gc[:], uc[:], ALU.mult, ["gc", "uc"], ["hid"])
        if ex + 1 < NE:
            load_x(ex + 1)
        for st_ in range(NST):
            yt = ysl[st_ % 2]
            for hf in range(2):
                for fc in range(8):
                    p.op("pe", lambda e, st_=st_, hf=hf, fc=fc, wi=wi: e.matmul(pd[hf][:], lhsT=hid[:, fc, st_ * 128:(st_ + 1) * 128], rhs=wdn[wi][:, fc, hf * 512:(hf + 1) * 512],
                                                                         start=(fc == 0), stop=(fc == 7)), reads=["hid", "wdn%d" % wi], writes=["pd%d" % hf])
                p.op("act", lambda e, hf=hf, yt=yt: e.activation(out=yt[:, hf * 512:(hf + 1) * 512], in_=pd[hf][:], func=AF.Copy), reads=["pd%d" % hf], writes=["ysl%d" % (st_ % 2)])
            p.dma("sp", Ys_d[ex * CAP + st_ * 128: ex * CAP + (st_ + 1) * 128, :], yt[:], reads=["ysl%d" % (st_ % 2)], writes=["Ys_d"])
        if ex + 1 < NE:
            cast_w(ex + 1)
    p.fence()
    p.flush()
    if stop == "p3":
        return nc
    P3.close()

    P4 = ExitStack()
    b_dn = load(P4, "b_dn", [32, 1024], F32, D["b_dn"])
    g_fin = load(P4, "g_fin", [128, 1024], F32, D["g_fin"])
    iota_i4 = p.sb(P4, "iota_i4", [128, 32], I32)
    iota_f4 = p.sb(P4, "iota_f4", [128, 32], F32)
    p.op("pool", lambda e: e.iota(iota_i4[:], pattern=[[1, 32]], base=0, channel_multiplier=0), writes=["iota_i4"])
    dv(lambda e: e.tensor_copy(out=iota_f4[:], in_=iota_i4[:]), ["iota_i4"], ["iota_f4"])
    rows = [p.sb(P4, "rows%d" % i, [128, 1024], F32) for i in range(4)]
    h2r = p.sb(P4, "h2r", [128, 1024], F32)
    acc = p.sb(P4, "acc", [128, 1024], F32)
    Gm = p.sb(P4, "Gm", [128, 32], F32)
    oh4 = p.sb(P4, "oh4", [128, 32], F32)
    GmT = p.sb(P4, "GmT", [32, 128], F32)
    junk4 = p.sb(P4, "junk4", [128, 1024], F32)
    ss4 = p.sb(P4, "ss4", [128, 1], F32)
    rstd4 = p.sb(P4, "rstd4", [128, 1], F32)
    ot = p.sb(P4, "ot", [128, 1024], F32)
    pb4 = [p.ps(P4, "pb4%d" % i, [128, 512], F32) for i in range(2)]
    pgt4 = p.ps(P4, "pgt4", [128, 512], F32)
    for ob in range(NB_OWN):
        p.dma("sp", h2r[:], h2_d[ob * 128:(ob + 1) * 128, :], reads=["h2_d"], writes=["h2r"])
        for k4 in range(4):
            p.dma("pool", None, None, reads=["slot_all", "Ys_d"], writes=["rows%d" % k4], stream="gath%d" % k4,
                  fn=lambda e, ob=ob, k4=k4: e.indirect_dma_start(out=rows[k4][:], out_offset=None, in_=Ys_d,
                                                                   in_offset=bass.IndirectOffsetOnAxis(ap=slot_all[:, ob, k4:k4 + 1], axis=0)))
        dv(lambda e: e.memset(Gm[:], 0.0), [], ["Gm"])
        for k4 in range(4):
            dv(lambda e, ob=ob, k4=k4: e.tensor_scalar(out=oh4[:], in0=iota_f4[:], scalar1=idx_all[:, ob, k4:k4 + 1], scalar2=gate_all[:, ob, k4:k4 + 1],
                                                       op0=ALU.is_equal, op1=ALU.mult), ["iota_f4", "idx_all", "gate_all"], ["oh4"])
            tt(Gm[:], Gm[:], oh4[:], ALU.add, ["Gm", "oh4"], ["Gm"])
        p.op("pe", lambda e: e.transpose(out=pgt4[0:32, 0:128], in_=Gm[:], identity=identf[:]), reads=["Gm", "identf"], writes=["pgt4"])
        p.op("act", lambda e: e.activation(out=GmT[:], in_=pgt4[0:32, 0:128], func=AF.Copy), reads=["pgt4"], writes=["GmT"])
        for hf in range(2):
            p.op("pe", lambda e, hf=hf: e.matmul(pb4[hf][:], lhsT=GmT[:], rhs=b_dn[:, hf * 512:(hf + 1) * 512], start=True, stop=True),
                 reads=["GmT", "b_dn"], writes=["pb4%d" % hf])
            tt(acc[:, hf * 512:(hf + 1) * 512], pb4[hf][:], h2r[:, hf * 512:(hf + 1) * 512], ALU.add, ["pb4%d" % hf, "h2r"], ["acc"])
        for k4 in range(4):
            dv(lambda e, ob=ob, k4=k4: e.scalar_tensor_tensor(out=acc[:], in0=rows[k4][:], scalar=gate_all[:, ob, k4:k4 + 1], in1=acc[:], op0=ALU.mult, op1=ALU.add),
               ["rows%d" % k4, "gate_all", "acc"], ["acc"])
        p.op("act", lambda e: e.activation(out=junk4[:], in_=acc[:], func=AF.Square, accum_out=ss4[:]), reads=["acc"], writes=["junk4", "ss4"])
        p.op("act", lambda e: e.activation(out=rstd4[:], in_=ss4[:], func=AF.Sqrt, scale=1.0 / 1024, bias=EPS), reads=["ss4"], writes=["rstd4"])
        dv(lambda e: e.reciprocal(out=rstd4[:], in_=rstd4[:]), ["rstd4"], ["rstd4"])
        dv(lambda e: e.scalar_tensor_tensor(out=ot[:], in0=acc[:], scalar=rstd4[:, 0:1], in1=g_fin[:], op0=ALU.mult, op1=ALU.mult), ["acc", "rstd4", "g_fin"], ["ot"])
        p.dma("sp", out[ob * 128:(ob + 1) * 128, :], ot[:], reads=["ot"], writes=["out"])
    p.fence()
    p.flush()
    P4.close()
    G.close()
    p.stack.close()
    return nc


_CACHE = {}


def kernel(**inputs):
    per = _host_prep(inputs)
    shapes = {k: v.shape for k, v in per[0].items()}
    nc = build_nc(shapes)
    res = run_bass_kernel_spmd(nc, per, core_ids=list(range(8)))
    outs = [np.asarray(r["out"], np.float32) for r in res.results]
    o = np.stack(outs, 0).reshape(2, 4 * NB_OWN * 128, 1024)
    return o
```
